# Optimizing a Trainium2 kernel written in Bass

```python
import jax, jax.numpy as jnp
from jax import lax
import numpy as np

D_MODEL = 1024
BATCH = 8
SEQ = 4096
DEPTH = 2

GRID_W = 64
CTX_LEN = 256

N_Q_HEADS = 8
N_KV_HEADS = 2
GQA_GROUP = N_Q_HEADS // N_KV_HEADS
HEAD_DIM = 64
ATTN_W = N_Q_HEADS * HEAD_DIM
KV_W = N_KV_HEADS * HEAD_DIM
ROPE_THETA = 10000.0
Q_BLOCK = 128
ATTN_SCALE = HEAD_DIM ** -0.5
CHUNK = 128
SG_GROUPS = 4
SG_GROUP_W = 128
SG_W = SG_GROUPS * SG_GROUP_W
FT_GROUPS = 4
FT_GROUP_W = 128
FT_W = FT_GROUPS * FT_GROUP_W
N_BRANCH = 3
BRANCH_W = 512
OFF_Q = 0
OFF_K = OFF_Q + ATTN_W
OFF_V = OFF_K + KV_W
OFF_U = OFF_V + KV_W
OFF_SGV = OFF_U + SG_W
OFF_FT = OFF_SGV + SG_W
OFF_GATE = OFF_FT + FT_W
IN_W = OFF_GATE + N_BRANCH * D_MODEL
D_FF = 2816
N_EXPERTS = 8
TOP_K = 2
D_FF_EXPERT = 3584
N_DENSE = (DEPTH + 1) // 2
N_MOE = DEPTH // 2
DEEPNORM_ALPHA = (2 * DEPTH) ** 0.25
DEEPNORM_BETA = (8 * DEPTH) ** -0.25
LN_EPS = 1e-6
RMS_EPS = 1e-6

kernel_name = "hybrid_gated_attn_sgmlp_fourier_moe_dit"


def _layer_norm(x):
    xf = x.astype(jnp.float32)
    mu = jnp.mean(xf, axis=-1, keepdims=True)
    var = jnp.mean(jnp.square(xf - mu), axis=-1, keepdims=True)
    return ((xf - mu) * lax.rsqrt(var + LN_EPS)).astype(x.dtype)


def _post_norm(x, y, g, b):
    r = DEEPNORM_ALPHA * x.astype(jnp.float32) + y.astype(jnp.float32)
    mu = jnp.mean(r, axis=-1, keepdims=True)
    var = jnp.mean(jnp.square(r - mu), axis=-1, keepdims=True)
    return ((r - mu) * lax.rsqrt(var + LN_EPS) * g + b).astype(x.dtype)


def _rms_norm(x, g):
    xf = x.astype(jnp.float32)
    return (xf * lax.rsqrt(jnp.mean(jnp.square(xf), axis=-1, keepdims=True) + RMS_EPS) * g).astype(x.dtype)


def _modulation(cond, w, b):
    m = jax.nn.silu(cond) @ w + b
    return [mi[..., None, :] for mi in jnp.split(m, 6, axis=-1)]


def _modulate(x, shift, scale):
    return _layer_norm(x) * (1 + scale) + shift


def _split_heads(z, n):
    return z.reshape(*z.shape[:-1], n, HEAD_DIM)


def _rope_1d(x, pos):
    d2 = x.shape[-1] // 2
    inv = ROPE_THETA ** (-jnp.arange(d2, dtype=jnp.float32) / d2)
    ang = pos.astype(jnp.float32)[:, None] * inv[None, :]
    cos = jnp.cos(ang)[:, None, :]
    sin = jnp.sin(ang)[:, None, :]
    xf = x.astype(jnp.float32)
    x1, x2 = xf[..., :d2], xf[..., d2:]
    return jnp.concatenate([x1 * cos - x2 * sin, x2 * cos + x1 * sin], axis=-1).astype(x.dtype)


def _axial_rope(x, t_row, t_col):
    half = HEAD_DIM // 2
    return jnp.concatenate([_rope_1d(x[..., :half], t_row), _rope_1d(x[..., half:], t_col)], axis=-1)


def _gqa_attend(q, k, v):
    s = jnp.einsum('bqkgd,btkd->bkgqt', q, k).astype(jnp.float32) * ATTN_SCALE
    p = jax.nn.softmax(s, axis=-1).astype(v.dtype)
    return jnp.einsum('bkgqt,btkd->bqkgd', p, v)


def _latent_attention(q, k_all, v_all):
    b, s = q.shape[:2]
    nb = s // Q_BLOCK
    qb = jnp.moveaxis(q.reshape(b, nb, Q_BLOCK, N_KV_HEADS, GQA_GROUP, HEAD_DIM), 1, 0)
    ob = lax.map(lambda blk: _gqa_attend(blk, k_all, v_all), qb)
    return jnp.moveaxis(ob, 0, 1).reshape(b, s, ATTN_W)


def _chunk_gating(u, v, w_s, b_s):
    b, l, _ = u.shape
    vg = _layer_norm(v.reshape(b, l, SG_GROUPS, SG_GROUP_W))
    vg = vg.reshape(b, l // CHUNK, CHUNK, SG_GROUPS, SG_GROUP_W)
    mixed = jnp.einsum('gpq,bnqgd->bnpgd', w_s, vg) + jnp.transpose(b_s)[:, :, None]
    return u * mixed.reshape(b, l, SG_W)


def _fourier_mix(f):
    b, l, _ = f.shape
    fg = f.reshape(b, l, FT_GROUPS, FT_GROUP_W).astype(jnp.float32)
    out = jnp.fft.fft2(fg, axes=(1, 3), norm='ortho').real
    return out.astype(f.dtype).reshape(b, l, FT_W)


def _merge_branches(z, y_attn, w_s, b_s, w_branch, w_out):
    y_sg = _chunk_gating(jax.nn.gelu(z[..., OFF_U:OFF_SGV]), jax.nn.gelu(z[..., OFF_SGV:OFF_FT]), w_s, b_s)
    y_ft = _fourier_mix(z[..., OFF_FT:OFF_GATE])
    y = jnp.stack([y_attn, y_sg, y_ft], axis=-2)
    p = jnp.einsum('blnc,ncd->blnd', y, w_branch)
    g = jax.nn.sigmoid(z[..., OFF_GATE:].astype(jnp.float32)).astype(z.dtype)
    g = g.reshape(*z.shape[:-1], N_BRANCH, D_MODEL)
    return jnp.sum(g * p, axis=-2) @ w_out


def _token_mixer(h_lat, h_ctx, w_in, q_g, k_g, w_s, b_s, w_branch, w_out, t_row, t_col, with_ctx_out):
    b, s, _ = h_lat.shape
    z = h_lat @ w_in
    q = _axial_rope(_rms_norm(_split_heads(z[..., OFF_Q:OFF_K], N_Q_HEADS), q_g), t_row, t_col)
    k = _axial_rope(_rms_norm(_split_heads(z[..., OFF_K:OFF_V], N_KV_HEADS), k_g), t_row, t_col)
    v = _split_heads(z[..., OFF_V:OFF_U], N_KV_HEADS)
    zc = h_ctx @ w_in if with_ctx_out else h_ctx @ w_in[:, OFF_K:OFF_U]
    base = 0 if with_ctx_out else OFF_K
    k_c = _rms_norm(_split_heads(zc[..., OFF_K - base:OFF_V - base], N_KV_HEADS), k_g)
    v_c = _split_heads(zc[..., OFF_V - base:OFF_U - base], N_KV_HEADS)
    k_all = jnp.concatenate([k_c, k], axis=1)
    v_all = jnp.concatenate([v_c, v], axis=1)
    y_lat = _merge_branches(z, _latent_attention(q, k_all, v_all), w_s, b_s, w_branch, w_out)
    y_ctx = None
    if with_ctx_out:
        cl = h_ctx.shape[1]
        q_c = _rms_norm(_split_heads(zc[..., OFF_Q:OFF_K], N_Q_HEADS), q_g)
        q_c = q_c.reshape(b, cl, N_KV_HEADS, GQA_GROUP, HEAD_DIM)
        y_attn_c = _gqa_attend(q_c, k_c, v_c).reshape(b, cl, ATTN_W)
        y_ctx = _merge_branches(zc, y_attn_c, w_s, b_s, w_branch, w_out)
    return y_lat, y_ctx


def _swiglu(x, wg, wu, wd):
    return (jax.nn.silu(x @ wg) * (x @ wu)) @ wd


def _moe_swiglu(x, router, wg, wu, wd):
    logits = (x @ router).astype(jnp.float32)
    top_v, top_i = lax.top_k(logits, TOP_K)
    top_w = jax.nn.softmax(top_v, axis=-1)
    gates = jnp.einsum('blk,blke->ble', top_w, jax.nn.one_hot(top_i, N_EXPERTS, dtype=jnp.float32)).astype(x.dtype)
    out = jnp.zeros_like(x)
    for e in range(N_EXPERTS):
        out = out + gates[..., e:e + 1] * _swiglu(x, wg[e], wu[e], wd[e])
    return out


def setup_inputs(seed: int = 0) -> dict:
    key = jax.random.key(seed)
    ks = jax.random.split(key, 24)

    def nrm(k, shape, scale):
        return jax.random.normal(k, shape, jnp.float32) * scale

    d = D_MODEL
    col_scale = jnp.ones((IN_W,), jnp.float32).at[OFF_V:OFF_V + KV_W].set(DEEPNORM_BETA)
    return {
        'x': nrm(ks[0], (BATCH, SEQ, d), 1.0),
        'c': nrm(ks[1], (BATCH, d), 1.0),
        'ctx': nrm(ks[2], (BATCH, CTX_LEN, d), 1.0),
        'c_ctx': nrm(ks[3], (d,), 1.0),
        'w_mod': nrm(ks[4], (DEPTH, d, 6 * d), d ** -0.5),
        'b_mod': nrm(ks[5], (DEPTH, 6 * d), 0.02),
        'w_in': nrm(ks[6], (DEPTH, d, IN_W), d ** -0.5) * col_scale,
        'q_norm': 1.0 + nrm(ks[7], (DEPTH, HEAD_DIM), 0.02),
        'k_norm': 1.0 + nrm(ks[8], (DEPTH, HEAD_DIM), 0.02),
        'sg_w': nrm(ks[9], (DEPTH, SG_GROUPS, CHUNK, CHUNK), CHUNK ** -0.5),
        'sg_b': 1.0 + nrm(ks[10], (DEPTH, SG_GROUPS, CHUNK), 0.02),
        'w_branch': nrm(ks[11], (DEPTH, N_BRANCH, BRANCH_W, d), BRANCH_W ** -0.5),
        'w_out': nrm(ks[12], (DEPTH, d, d), d ** -0.5 * DEEPNORM_BETA),
        'ln1_g': 1.0 + nrm(ks[13], (DEPTH, d), 0.02),
        'ln1_b': nrm(ks[14], (DEPTH, d), 0.02),
        'ln2_g': 1.0 + nrm(ks[15], (DEPTH, d), 0.02),
        'ln2_b': nrm(ks[16], (DEPTH, d), 0.02),
        'ffn_w_gate': nrm(ks[17], (N_DENSE, d, D_FF), d ** -0.5),
        'ffn_w_up': nrm(ks[18], (N_DENSE, d, D_FF), d ** -0.5 * DEEPNORM_BETA),
        'ffn_w_down': nrm(ks[19], (N_DENSE, D_FF, d), D_FF ** -0.5 * DEEPNORM_BETA),
        'router': nrm(ks[20], (N_MOE, d, N_EXPERTS), d ** -0.5),
        'exp_w_gate': nrm(ks[21], (N_MOE, N_EXPERTS, d, D_FF_EXPERT), d ** -0.5),
        'exp_w_up': nrm(ks[22], (N_MOE, N_EXPERTS, d, D_FF_EXPERT), d ** -0.5 * DEEPNORM_BETA),
        'exp_w_down': nrm(ks[23], (N_MOE, N_EXPERTS, D_FF_EXPERT, d), D_FF_EXPERT ** -0.5 * DEEPNORM_BETA),
    }


def reference(x, c, ctx, c_ctx, w_mod, b_mod, w_in, q_norm, k_norm, sg_w, sg_b, w_branch, w_out,
              ln1_g, ln1_b, ln2_g, ln2_b, ffn_w_gate, ffn_w_up, ffn_w_down,
              router, exp_w_gate, exp_w_up, exp_w_down):
    s = x.shape[1]
    rows = s // GRID_W
    t_row = jnp.repeat(jnp.arange(rows, dtype=jnp.int32), GRID_W)
    t_col = jnp.tile(jnp.arange(GRID_W, dtype=jnp.int32), rows)
    x_lat, x_ctx = x, ctx
    for l in range(DEPTH):
        last = l == DEPTH - 1
        m_lat = _modulation(c, w_mod[l], b_mod[l])
        m_ctx = _modulation(c_ctx, w_mod[l], b_mod[l])
        h_lat = _modulate(x_lat, m_lat[0], m_lat[1])
        h_ctx = _modulate(x_ctx, m_ctx[0], m_ctx[1])
        y_lat, y_ctx = _token_mixer(h_lat, h_ctx, w_in[l], q_norm[l], k_norm[l], sg_w[l], sg_b[l],
                                    w_branch[l], w_out[l], t_row, t_col, not last)
        x_lat = _post_norm(x_lat, m_lat[2] * y_lat, ln1_g[l], ln1_b[l])
        if not last:
            x_ctx = _post_norm(x_ctx, m_ctx[2] * y_ctx, ln1_g[l], ln1_b[l])
        h_lat = _modulate(x_lat, m_lat[3], m_lat[4])
        if l % 2 == 0:
            i = l // 2
            f_lat = _swiglu(h_lat, ffn_w_gate[i], ffn_w_up[i], ffn_w_down[i])
            if not last:
                f_ctx = _swiglu(_modulate(x_ctx, m_ctx[3], m_ctx[4]), ffn_w_gate[i], ffn_w_up[i], ffn_w_down[i])
        else:
            i = l // 2
            f_lat = _moe_swiglu(h_lat, router[i], exp_w_gate[i], exp_w_up[i], exp_w_down[i])
            if not last:
                f_ctx = _moe_swiglu(_modulate(x_ctx, m_ctx[3], m_ctx[4]), router[i], exp_w_gate[i], exp_w_up[i], exp_w_down[i])
        x_lat = _post_norm(x_lat, m_lat[5] * f_lat, ln2_g[l], ln2_b[l])
        if not last:
            x_ctx = _post_norm(x_ctx, m_ctx[5] * f_ctx, ln2_g[l], ln2_b[l])
    return x_lat
```

```python
import contextlib
import numpy as np
import ml_dtypes
import concourse.bass as bass
import concourse.mybir as mybir
from concourse.bass_utils import run_bass_kernel_spmd

F32 = mybir.dt.float32
BF16 = mybir.dt.bfloat16
ALU = mybir.AluOpType
AF = mybir.ActivationFunctionType

ENGS = ("pe", "act", "dve", "pool", "sp")


class Op:
    __slots__ = ("eng", "fn", "reads", "writes", "dma", "ndma", "key", "deps",
                 "needs_inc", "sem", "val", "waits", "extra")

    def __init__(self, eng, fn, reads, writes, dma=False, ndma=1, key=None):
        self.eng = eng
        self.fn = fn
        self.reads = tuple(reads)
        self.writes = tuple(writes)
        self.dma = dma
        self.ndma = ndma
        self.key = key
        self.deps = ()
        self.needs_inc = False
        self.sem = None
        self.val = 0
        self.waits = ()
        self.extra = ()


def is_psum(t):
    return t in ("po", "pbc") or (isinstance(t, tuple) and len(t) == 2 and t[0] in ("ps", "pt"))


class Prog:
    def __init__(self, nc):
        self.nc = nc
        self.ops = []
        self.last_eng = {}
        self.last_key = {}
        self.bar = ()
        self.bar_pending = set()

    def barrier(self):
        self.bar = tuple(self.last_eng.values()) + tuple(self.last_key.values())
        self.bar_pending = set(ENGS)

    def _track(self, o):
        i = len(self.ops)
        if o.eng in self.bar_pending:
            o.extra = self.bar
            self.bar_pending.discard(o.eng)
        self.ops.append(o)
        if o.dma:
            self.last_key[o.key] = i
        else:
            self.last_eng[o.eng] = i

    def I(self, eng, method, *args, reads=(), writes=(), **kw):
        return self.M(eng, [(method, args, kw)], reads, writes)

    def M(self, eng, insts, reads=(), writes=()):
        insts = list(insts)

        def fn(e):
            r = None
            for (m, a, kw) in insts:
                r = getattr(e, m)(*a, **kw)
            return r
        o = Op(eng, fn, reads, writes)
        self._track(o)
        return o

    def D(self, eng, pairs, key, reads=(), writes=()):
        pairs = list(pairs)

        def fn(e):
            return [e.dma_start(out=o_, in_=i_) for (o_, i_) in pairs]
        o = Op(eng, fn, reads, tuple(writes) + (("dmakey", key),), dma=True, ndma=len(pairs), key=key)
        self._track(o)
        return o

    def analyze(self):
        last_w = {}
        readers = {}
        ops = self.ops
        for i, o in enumerate(ops):
            deps = set()
            for t in o.reads:
                w = last_w.get(t)
                if w is not None:
                    deps.add(w)
                if is_psum(t):
                    for r in readers.get(t, ()):
                        if ops[r].eng != o.eng:
                            deps.add(r)
            for t in o.writes:
                w = last_w.get(t)
                if w is not None:
                    deps.add(w)
                for r in readers.get(t, ()):
                    deps.add(r)
            deps.update(o.extra)
            deps.discard(i)
            keep = []
            for j in deps:
                d = ops[j]
                if d.dma:
                    keep.append(j)
                    continue
                if d.eng == o.eng and not o.dma:
                    if o.eng == "pe":
                        continue
                keep.append(j)
            o.deps = keep
            for j in keep:
                ops[j].needs_inc = True
            for t in o.reads:
                readers.setdefault(t, []).append(i)
            for t in o.writes:
                last_w[t] = i
                readers[t] = []

    def emit(self, final_waits=()):
        nc = self.nc
        self.analyze()
        ops = self.ops
        keys = []
        seen = set()
        for o in ops:
            if o.dma and o.key not in seen:
                seen.add(o.key)
                keys.append(o.key)
        with contextlib.ExitStack() as es:
            esem = {e: es.enter_context(nc.semaphore("s_" + e)) for e in ENGS}
            ksem = {k: es.enter_context(nc.semaphore("k%d" % n)) for n, k in enumerate(keys)}
            cnt = {e: 0 for e in ENGS}
            kcnt = {k: 0 for k in keys}
            for o in ops:
                if o.dma:
                    kcnt[o.key] += 16 * o.ndma
                    o.sem = ksem[o.key]
                    o.val = kcnt[o.key]
                elif o.needs_inc:
                    cnt[o.eng] += 1
                    o.sem = esem[o.eng]
                    o.val = cnt[o.eng]
            known = {e: {} for e in ENGS}
            for o in ops:
                w = {}
                for j in o.deps:
                    d = ops[j]
                    sid = id(d.sem)
                    if sid not in w or w[sid][1] < d.val:
                        w[sid] = (d.sem, d.val)
                kn = known[o.eng]
                ws = []
                for sid, (s, v) in w.items():
                    if kn.get(sid, 0) >= v:
                        continue
                    kn[sid] = v
                    ws.append((s, v))
                o.waits = ws
            per_eng = {e: [o for o in ops if o.eng == e] for e in ENGS}
            last_dma = {}
            for o in ops:
                if o.dma:
                    last_dma[o.key] = o
            block = es.enter_context(nc.Block())

            def run(eng_name, eng):
                for o in per_eng[eng_name]:
                    for s, v in o.waits:
                        eng.wait_ge(s, v)
                    r = o.fn(eng)
                    if o.dma:
                        rs = r if isinstance(r, (list, tuple)) else [r]
                        assert len(rs) == o.ndma, (len(rs), o.ndma)
                        for ins in rs:
                            ins.then_inc(o.sem, 16)
                    elif o.needs_inc:
                        r.then_inc(o.sem, 1)
                if eng_name == "sp":
                    for k in (final_waits if final_waits else list(last_dma.keys())):
                        if k in last_dma:
                            o = last_dma[k]
                            eng.wait_ge(o.sem, o.val)

            @block.tensor
            def _(e):
                run("pe", e)

            @block.scalar
            def _(e):
                run("act", e)

            @block.vector
            def _(e):
                run("dve", e)

            @block.gpsimd
            def _(e):
                run("pool", e)

            @block.sync
            def _(e):
                run("sp", e)
        return {e: len(per_eng[e]) for e in ENGS}, len(keys)


class Cfg:
    def __init__(self, D=1024, S=4096, CTX=256, DFF=2816, NE=8, DFFE=3584, DEPTH=2, GRID_W=64, SL=4):
        self.D, self.S, self.CTX, self.DFF, self.NE, self.DFFE = D, S, CTX, DFF, NE, DFFE
        self.DEPTH, self.GRID_W, self.SL = DEPTH, GRID_W, SL
        self.KD = D // 128
        self.NT = S // 128
        self.NCT = CTX // 128
        self.TOT = S + CTX
        self.TT = self.NT + self.NCT
        self.OFF_Q, self.OFF_K, self.OFF_V, self.OFF_U = 0, 512, 640, 768
        self.OFF_SGV, self.OFF_FT, self.OFF_GATE = 1280, 1792, 2304
        self.INW = 2304 + 3 * D
        self.ALPHA = float((2 * DEPTH) ** 0.25)
        self.BW = 512
        self.NKB = S // 256
        self.ARENA_KB = 166
        self.EMIT_LAYERS = DEPTH
        self.STOP = None
        self.MAXOPS = None


FULL = Cfg()


def make_consts(cfg):
    bf = ml_dtypes.bfloat16
    S, CTX, TOT, GW = cfg.S, cfg.CTX, cfg.TOT, cfg.GRID_W
    c = {}
    c["identb"] = np.eye(128, dtype=np.float32).astype(bf)
    P = np.zeros((128, 128), np.float32)
    for m in range(128):
        if (m % 32) < 16:
            P[m + 16, m] = -1.0
        else:
            P[m - 16, m] = 1.0
    c["rotP"] = P.astype(bf)
    B = np.zeros((128, 128), np.float32)
    B[:64, :64] = 1.0
    B[64:, 64:] = 1.0
    c["blockones"] = B.astype(bf)
    sel = np.zeros((128, 128), np.float32)
    sel[64, 0:64] = 1.0
    sel[0, 64:128] = 1.0
    c["onesb"] = sel.astype(bf)
    t = np.arange(S)
    row = (t // GW).astype(np.float64)
    col = (t % GW).astype(np.float64)
    i = np.arange(128) % 64
    j = i % 16
    inv = 10000.0 ** (-(j.astype(np.float64)) / 16.0)
    pos = np.where((i < 32)[:, None], row[None, :], col[None, :])
    ang = pos * inv[:, None]
    cs = np.zeros((2, 128, TOT), np.float32)
    cs[0, :, :S] = np.cos(ang)
    cs[1, :, :S] = np.sin(ang)
    cs[0, :, S:] = 1.0
    c["ropecs"] = cs

    def dft_table(L, nkb, kw):
        l = np.arange(L, dtype=np.int64)
        k = np.arange(L, dtype=np.int64)
        ph = (np.outer(l, k) % L).astype(np.float64) * (2.0 * np.pi / L)
        C = np.cos(ph).astype(np.float32)
        Sn = np.sin(ph).astype(np.float32)
        tab = np.stack([C, Sn], axis=1)
        tab = tab.reshape(L // 128, 128, 2, nkb, kw).transpose(0, 3, 1, 2, 4)
        return np.ascontiguousarray(tab).astype(bf)

    c["dftL"] = dft_table(S, cfg.NKB, 256)
    c["dftC"] = dft_table(CTX, 1, CTX)
    d = np.arange(128, dtype=np.int64)
    ph = (np.outer(d, d) % 128).astype(np.float64) * (2.0 * np.pi / 128)
    c["c128"] = np.stack([np.cos(ph), -np.sin(ph)], axis=1).astype(np.float32).astype(bf)
    return c


def build_program(cfg, debug_outs=()):
    nc = bass.Bass("TRN2", target_bir_lowering=False)
    D, S, CTX, TOT, KD, NT, NCT, TT = cfg.D, cfg.S, cfg.CTX, cfg.TOT, cfg.KD, cfg.NT, cfg.NCT, cfg.TT
    L, NE, INW = cfg.DEPTH, cfg.NE, cfg.INW
    ND = (L + 1) // 2
    NM = L // 2

    def din(name, shape, dt=F32):
        return nc.dram_tensor(name, list(shape), dt, kind="ExternalInput").ap()

    def dscr(name, shape, dt):
        kind = "ExternalOutput" if name in debug_outs else "Internal"
        return nc.dram_tensor(name, list(shape), dt, kind=kind).ap()

    x_in = din("x", [S, D])
    ctx_in = din("ctx", [CTX, D])
    cvec = din("cvecs", [128, KD, 2])
    w_mod = din("w_mod", [L, D, 6 * D])
    b_modF = din("b_modF", [L, 128, 6 * KD])
    b_mod = din("b_mod", [L, 6 * D])
    w_in = din("w_in", [L, D, INW])
    qk_norm = din("qk_norm", [L, 128, 2])
    sg_wT = din("sg_wT", [L, 128, 4, 128])
    sg_b = din("sg_b", [L, 512])
    w_branch = din("w_branch", [L, 3, 512, D])
    w_out = din("w_out", [L, D, D])
    ln_gb = din("ln_gb", [L, 4, D])
    ffn_wg = din("ffn_w_gate", [ND, D, cfg.DFF])
    ffn_wu = din("ffn_w_up", [ND, D, cfg.DFF])
    ffn_wd = din("ffn_w_down", [ND, cfg.DFF, D])
    if NM:
        router = din("router", [NM, D, NE])
        exp_wg = din("exp_w_gate", [NM, NE, D, cfg.DFFE])
        exp_wu = din("exp_w_up", [NM, NE, D, cfg.DFFE])
        exp_wd = din("exp_w_down", [NM, NE, cfg.DFFE, D])
    identb_d = din("identb", [128, 128], BF16)
    rotP_d = din("rotP", [128, 128], BF16)
    blockones_d = din("blockones", [128, 128], BF16)
    onesb_d = din("onesb", [128, 128], BF16)
    ropecs_d = din("ropecs", [2, 128, TOT])
    dftL_d = din("dftL", [NT, cfg.NKB, 128, 2, 256], BF16)
    dftC_d = din("dftC", [NCT, 1, 128, 2, CTX], BF16)
    c128_d = din("c128", [128, 2, 128], BF16)
    out_d = nc.dram_tensor("out", [S, D], F32, kind="ExternalOutput").ap()

    hT_d = dscr("hT_d", [128, KD, TOT], BF16)
    ftok_d = dscr("ftok_d", [TT, 128, 512], BF16)
    yb_d = [dscr("y%d_d" % n, [128, 4, TOT], BF16) for n in range(3)]
    x1_d = dscr("x1_d", [TOT, D], F32)
    x2_d = [dscr("x2_d%d" % l, [TOT, D], F32) for l in range(max(L - 1, 1))]

    P = Prog(nc)
    es = contextlib.ExitStack()
    with es:
        def sb(name, shape, dt):
            return es.enter_context(nc.sbuf_tensor("s_" + name, list(shape), dt))

        PB = [es.enter_context(nc.psum_tensor("pb%d" % i, [128, 512], F32)) for i in range(6)]
        PT = [es.enter_context(nc.psum_tensor("pt%d" % i, [128, 1024], BF16)) for i in range(2)]
        PTF = [PT[i].bitcast(F32) for i in range(2)]
        ring_state = {"i": 0}

        def ps_next():
            i = ring_state["i"]
            ring_state["i"] = (i + 1) % 4
            return PB[i], ("ps", i)

        pt_state = {"i": 0}

        def pt_next():
            i = pt_state["i"]
            pt_state["i"] = 1 - i
            return PT[i][:, 0:512], ("pt", i)

        class Ring:
            def __init__(self, name, n, shape, dt):
                self.t = [sb("%s%d" % (name, i), shape, dt) for i in range(n)]
                self.name = name
                self.n = n
                self.i = 0

            def next(self):
                i = self.i
                self.i = (i + 1) % self.n
                return self.t[i], (self.name, i)

        identb = sb("identb", [128, 128], BF16)
        rotP = sb("rotP", [128, 128], BF16)
        blockones = sb("blockones", [128, 128], BF16)
        selb = sb("onesb", [128, 128], BF16)
        c128 = sb("c128", [128, 2, 128], BF16)
        epsb = sb("epsb", [128, 1], F32)
        zeros = sb("zeros", [128, 128], F32)
        for nm, t, d in (("identb", identb, identb_d), ("rotP", rotP, rotP_d), ("blockones", blockones, blockones_d),
                         ("selb", selb, onesb_d), ("c128", c128, c128_d)):
            P.D("sp", [(t[:], d)], key=nm, writes=[nm])
        P.I("pool", "memset", epsb[:], 1e-6, writes=["epsb"])
        P.I("pool", "memset", zeros[:], 0.0, writes=["zeros"])

        cv32 = sb("cv32", [128, KD, 2], F32)
        scT = sb("scT", [128, KD, 2], BF16)
        sc32 = sb("sc32", [128, KD, 2], F32)
        modF = sb("modF", [128, 6 * KD, 2], F32)
        bmF = sb("bmF", [128, 6 * KD], F32)
        bcg = sb("bcg", [128, 4, D], F32)
        lngb = sb("lngb", [128, 4, D], F32)
        qkn = sb("qkn", [128, 2], F32)

        P.D("sp", [(cv32[:], cvec)], key="cv32", writes=["cv32"])
        P.I("act", "activation", sc32[:], cv32[:], AF.Silu, reads=["cv32"], writes=["sc32"])
        P.I("dve", "tensor_copy", scT[:], sc32[:], reads=["sc32"], writes=["scT"])

        ARENA_BYTES = cfg.ARENA_KB * 1024
        arena_f = sb("arena", [128, ARENA_BYTES // 4], F32)
        arena_b = arena_f.bitcast(BF16)
        ar = {"off": 0}
        R = {}
        T = {}

        def a_alloc(shape, dt):
            n = 1
            for d_ in shape[1:]:
                n *= d_
            esz = 4 if dt == F32 else 2
            off = ar["off"]
            nbytes = (n * esz + 63) // 64 * 64
            assert off + nbytes <= ARENA_BYTES, ("arena overflow", off, nbytes, ARENA_BYTES)
            ar["off"] = off + nbytes
            base = arena_f if dt == F32 else arena_b
            v = base[:, off // esz:off // esz + n]
            if len(shape) == 3:
                v = v.rearrange("p (a b) -> p a b", a=shape[1])
            elif len(shape) == 4:
                v = v.rearrange("p (a b c) -> p a b c", a=shape[1], b=shape[2])
            return v

        class Ring:
            def __init__(self, name, n, shape, dt):
                self.t = [a_alloc(shape, dt) for i in range(n)]
                self.name = name
                self.n = n
                self.i = 0

            def next(self):
                i = self.i
                self.i = (i + 1) % self.n
                return self.t[i], (self.name, i)

        SPEC = {
            "wm": (2, [128, KD, 512], BF16), "xt": (2, [128, D], F32), "xn4": (2, [128, 4, D], BF16),
            "hTb": (2, [128, KD, 512], BF16), "stt": (4, [128, 8, 6], F32), "mv": (4, [128, 4, 2], F32),
            "sd": (4, [128, 4], F32), "rs": (4, [128, 4], F32),
            "tmpA": (2, [128, 512], F32), "tmpB": (2, [128, 512], F32), "tmpC": (2, [128, 512], F32),
            "tbf": (2, [128, 512], BF16), "tbf2": (2, [128, 512], BF16), "rcs": (2, [128, 2, 512], F32),
            "r": (2, [128, D], F32), "xo": (2, [128, D], F32),
            "wgu": (2, [128, KD, 2, cfg.SL * 128], BF16), "wd": (2, [128, cfg.SL, D], BF16), "actT": (2, [128, cfg.SL, 512], BF16),
            "yblk": (2, [128, 4, 512], BF16), "qT": (8, [128, 512], BF16), "pT": (4, [128, 512], BF16),
            "mx": (2, [128, 4, 512], F32), "gv": (2, [128, 512], F32), "vg": (2, [128, 512], BF16),
            "dft": (4, [128, 2, 256], BF16), "yfr": (2, [128, 4, 256], BF16),
            "mT": (1, [128, KD, 512], BF16), "ybs0": (1, [128, 4, 512], BF16), "ybs1": (1, [128, 4, 512], BF16),
            "ybs2": (1, [128, 4, 512], BF16), "acc": (2, [128, 512], F32),
        }

        def make_ring(nm):
            cnt = None
            if isinstance(nm, tuple):
                nm, cnt = nm
            n_, shp, dt = SPEC[nm]
            R[nm] = Ring(nm, cnt or n_, shp, dt)

        def phase(rings=(), tensors=(), keep=0):
            P.barrier()
            ar["off"] = keep
            for k_ in list(R.keys()):
                del R[k_]
            for nm, shp, dt in tensors:
                T[nm] = a_alloc(shp, dt)
            for nm in rings:
                make_ring(nm)

        LN_RINGS = ("stt", "mv", "sd", "rs")
        HT_RINGS = (("xt", 4), "xn4", "hTb") + LN_RINGS

        NF = D // 512

        def mm(out_ap, pairs, reads, writes):
            pairs = list(pairs)
            n_ = len(pairs)
            P.M("pe", [("matmul", (out_ap, a, b), dict(start=(i == 0), stop=(i == n_ - 1))) for i, (a, b) in enumerate(pairs)],
                reads, writes)

        def ln_multi(items):
            bufs = []
            for it in items:
                bufs.append((R["stt"].next(), R["mv"].next(), R["sd"].next(), R["rs"].next()))
            for (src_ap, src_tok, dst_ap, dst_tok), ((st, stt), (mv, mvt), (sd, sdt), (rs, rst)) in zip(items, bufs):
                P.M("dve", [("bn_stats", (st[:, f, :], src_ap[:, f * 512:(f + 1) * 512]), {}) for f in range(NF)], reads=[src_tok], writes=[stt])
            for (src_ap, src_tok, dst_ap, dst_tok), ((st, stt), (mv, mvt), (sd, sdt), (rs, rst)) in zip(items, bufs):
                P.I("dve", "bn_aggr", mv[:, 0, :], st[:, 0:NF, :].rearrange("p g s -> p (g s)"), reads=[stt], writes=[mvt])
            for (src_ap, src_tok, dst_ap, dst_tok), ((st, stt), (mv, mvt), (sd, sdt), (rs, rst)) in zip(items, bufs):
                P.I("act", "activation", sd[:, 0:1], mv[:, 0, 1:2], AF.Sqrt, bias=epsb[:], reads=[mvt, "epsb"], writes=[sdt])
            for (src_ap, src_tok, dst_ap, dst_tok), ((st, stt), (mv, mvt), (sd, sdt), (rs, rst)) in zip(items, bufs):
                P.I("dve", "reciprocal", rs[:, 0:1], sd[:, 0:1], reads=[sdt], writes=[rst])
            for (src_ap, src_tok, dst_ap, dst_tok), ((st, stt), (mv, mvt), (sd, sdt), (rs, rst)) in zip(items, bufs):
                P.I("dve", "tensor_scalar", dst_ap, src_ap, mv[:, 0, 0:1], rs[:, 0:1], ALU.subtract, ALU.mult,
                    reads=[src_tok, mvt, rst], writes=[dst_tok])

        def x_src(l, tile):
            if l == 0:
                if tile < NT:
                    return x_in[tile * 128:(tile + 1) * 128, :], None
                return ctx_in[(tile - NT) * 128:(tile - NT + 1) * 128, :], None
            return x2_d[l - 1][tile * 128:(tile + 1) * 128, :], ("x2_d", l - 1, tile)

        def make_hT(blk, src_fn, shift_j, scale_j):
            t0, n, s_ = blk
            nt = n // 128
            xn, xnt = R["xn4"].next()
            items = []
            for ti in range(nt):
                tile = t0 // 128 + ti
                src, srctok = src_fn(tile)
                xt, xtt = R["xt"].next()
                P.D("sp", [(xt[:], src)], key=xtt, reads=[srctok] if srctok else [], writes=[xtt])
                items.append((xt[:], xtt, xn[:, ti, :], (xnt, ti)))
            ln_multi(items)
            hTb, hTt = R["hTb"].next()
            for k in range(KD):
                pt, ptt = pt_next()
                P.M("pe", [("transpose", (pt[:, ti * 128:(ti + 1) * 128], xn[:, ti, k * 128:(k + 1) * 128], identb[:]), {}) for ti in range(nt)],
                    reads=[(xnt, ti) for ti in range(nt)] + ["identb"], writes=[ptt])
                P.I("dve", "tensor_scalar", hTb[:, k, 0:n], pt[:, 0:n],
                    modF[:, scale_j + k, s_:s_ + 1], modF[:, shift_j + k, s_:s_ + 1], ALU.mult, ALU.add,
                    reads=[ptt, "modF"], writes=[(hTt, k)])
            return hTb, [(hTt, k) for k in range(KD)]

        def gelu(src_ps, src_tok, dst_ap, dst_tok, n, extra_mul=None, extra_toks=()):
            a, at = R["tmpA"].next()
            b, bt = R["tmpB"].next()
            P.I("act", "activation", a[:, 0:n], src_ps, AF.Square, reads=[src_tok], writes=[at])
            P.I("dve", "tensor_scalar", a[:, 0:n], a[:, 0:n], 0.044715, 1.0, ALU.mult, ALU.add, reads=[at], writes=[at])
            P.I("dve", "tensor_tensor", a[:, 0:n], a[:, 0:n], src_ps, ALU.mult, reads=[at, src_tok], writes=[at])
            P.I("act", "activation", b[:, 0:n], a[:, 0:n], AF.Sigmoid, scale=1.5957691216057308, reads=[at], writes=[bt])
            if extra_mul is None:
                P.I("dve", "tensor_tensor", dst_ap, b[:, 0:n], src_ps, ALU.mult, reads=[bt, src_tok], writes=[dst_tok])
            else:
                P.I("dve", "tensor_tensor", b[:, 0:n], b[:, 0:n], src_ps, ALU.mult, reads=[bt, src_tok], writes=[bt])
                P.I("dve", "tensor_tensor", dst_ap, b[:, 0:n], extra_mul, ALU.mult, reads=[bt] + list(extra_toks), writes=[dst_tok])

        def rope_norm(zps, ztok, gcol, t0, n, dst_ap, dst_tok):
            sq, sqt = R["tbf"].next()
            zg, zgt = R["tbf2"].next()
            P.I("act", "activation", sq[:, 0:n], zps, AF.Square, reads=[ztok], writes=[sqt])
            P.I("act", "activation", zg[:, 0:n], zps, AF.Copy, scale=qkn[:, gcol:gcol + 1], reads=[ztok, "qkn"], writes=[zgt])
            pa, pat = ps_next()
            pb, pbt = ps_next()
            mm(pa[:, 0:n], [(blockones[:], sq[:, 0:n])], reads=[sqt, "blockones"], writes=[pat])
            mm(pb[:, 0:n], [(rotP[:], zg[:, 0:n])], reads=[zgt, "rotP"], writes=[pbt])
            cs, cst = R["rcs"].next()
            P.D("sp", [(cs[:, c_, 0:n], ropecs_d[c_, :, t0:t0 + n]) for c_ in range(2)], key=cst, writes=[cst])
            a, at = R["tmpA"].next()
            b, bt = R["tmpB"].next()
            c2, ct = R["tmpC"].next()
            P.I("act", "activation", a[:, 0:n], pa[:, 0:n], AF.Sqrt, bias=epsb[:], scale=1.0 / 64.0, reads=[pat, "epsb"], writes=[at])
            P.I("dve", "reciprocal", a[:, 0:n], a[:, 0:n], reads=[at], writes=[at])
            P.I("dve", "tensor_tensor", b[:, 0:n], zg[:, 0:n], cs[:, 0, 0:n], ALU.mult, reads=[zgt, cst], writes=[bt])
            P.I("dve", "tensor_tensor", c2[:, 0:n], pb[:, 0:n], cs[:, 1, 0:n], ALU.mult, reads=[pbt, cst], writes=[ct])
            P.I("dve", "tensor_tensor", b[:, 0:n], b[:, 0:n], c2[:, 0:n], ALU.add, reads=[bt, ct], writes=[bt])
            P.I("dve", "tensor_tensor", dst_ap, b[:, 0:n], a[:, 0:n], ALU.mult, reads=[bt, at], writes=[dst_tok])

        def wload(dst, dtok, src_ap, key):
            P.D("pool", [(dst, src_ap)], key=key, writes=[dtok])

        def post_norm_multi(tiles, gate_idx, g_idx, b_idx):
            st = []
            for (y_fn, dst_ap, dst_tok_d, src_ap, src_tok_d) in tiles:
                r, rt = R["r"].next()
                xt, xtt = R["xt"].next()
                xo, xot = R["xo"].next()
                P.D("sp", [(xt[:], src_ap)], key=xtt, reads=[src_tok_d] if src_tok_d else [], writes=[xtt])
                st.append((r, rt, xt, xtt, xo, xot))
            for (y_fn, dst_ap, dst_tok_d, src_ap, src_tok_d), (r, rt, xt, xtt, xo, xot) in zip(tiles, st):
                for cb in range(NF):
                    yap, ytok = y_fn(cb)
                    P.I("dve", "tensor_tensor", r[:, cb * 512:(cb + 1) * 512], yap, bcg[:, gate_idx, cb * 512:(cb + 1) * 512], ALU.mult,
                        reads=[ytok, "bcg"], writes=[(rt, cb)])
            for (r, rt, xt, xtt, xo, xot) in st:
                P.I("dve", "scalar_tensor_tensor", r[:], xt[:], cfg.ALPHA, r[:], ALU.mult, ALU.add,
                    reads=[xtt] + [(rt, cb) for cb in range(NF)], writes=[rt])
            ln_multi([(r[:], rt, xo[:], xot) for (r, rt, xt, xtt, xo, xot) in st])
            for (r, rt, xt, xtt, xo, xot) in st:
                P.I("dve", "tensor_tensor", xo[:], xo[:], lngb[:, g_idx, :], ALU.mult, reads=[xot, "lngb"], writes=[xot])
            for (r, rt, xt, xtt, xo, xot) in st:
                P.I("pool", "tensor_tensor", xo[:], xo[:], lngb[:, b_idx, :], ALU.add, reads=[xot, "lngb"], writes=[xot])
            for (y_fn, dst_ap, dst_tok_d, src_ap, src_tok_d), (r, rt, xt, xtt, xo, xot) in zip(tiles, st):
                P.D("pool", [(dst_ap, xo[:])], key=xot, reads=[xot], writes=[dst_tok_d])

        lat_blocks = [(b * 512, 512, 0) for b in range(S // 512)]
        ctx_block = (S, CTX, 1)

        SL = cfg.SL
        HALF_T = max(NT // 2, NCT)
        all_k = [("kT", h, b[0]) for h in range(2) for b in lat_blocks + [ctx_block]]

        def load_hT(blk):
            t0, n, s_ = blk
            hTb, hTt = R["hTb"].next()
            P.D("sp", [(hTb[:, :, 0:n], hT_d[:, :, t0:t0 + n])], key=("hTld", hTt),
                reads=[("hT_d", t0)], writes=[(hTt, k) for k in range(KD)])
            return hTb, [(hTt, k) for k in range(KD)]

        def modulation(l):
            scB, bmbc = T["scB"], T["bmbc"]
            for s_ in range(2):
                for k in range(KD):
                    P.I("dve", "tensor_scalar", scB[:, s_, k, :], zeros[:], sc32[:, k, s_:s_ + 1], None, ALU.add,
                        reads=["zeros", "sc32"], writes=[("scB", s_, k)])
            P.D("sp", [(bmF[:], b_modF[l])], key="bmF", writes=["bmF"])
            P.D("sp", [(lngb[:], ln_gb[l:l + 1].broadcast_to([128, 4, D]))], key="lngb", writes=["lngb"])
            P.D("sp", [(bmbc[:, 0, :], b_mod[l:l + 1, 2 * D:3 * D].broadcast_to([128, D])),
                       (bmbc[:, 1, :], b_mod[l:l + 1, 5 * D:6 * D].broadcast_to([128, D]))], key="bmbc", writes=["bmbc"])
            P.D("sp", [(qkn[:], qk_norm[l])], key="qkn", writes=["qkn"])
            wmv = w_mod[l].rearrange("(k p) n -> p k n", p=128)
            nslab = 6 * D // 512
            for sl in range(nslab):
                wm, wmt = R["wm"].next()
                wload(wm[:], wmt, wmv[:, :, sl * 512:(sl + 1) * 512], key=wmt)
                part = (sl * 512) // D
                if part in (2, 5):
                    gi = 0 if part == 2 else 1
                    col0 = sl * 512 - part * D
                    for s_ in range(2):
                        pb, pbt = ps_next()
                        mm(pb[:], [(scB[:, s_, k, :], wm[:, k, :]) for k in range(KD)],
                           reads=[wmt] + [("scB", s_, k) for k in range(KD)], writes=[pbt])
                        P.I("dve", "tensor_tensor", bcg[:, gi * 2 + s_, col0:col0 + 512], pb[:], bmbc[:, gi, col0:col0 + 512], ALU.add,
                            reads=[pbt, "bmbc"], writes=["bcg"])
                else:
                    pb, pbt = ps_next()
                    j0 = sl * 4
                    P.M("pe", [("matmul", (pb[:, jj * 2:jj * 2 + 2], wm[:, k, jj * 128:(jj + 1) * 128], scT[:, k, :]),
                                dict(start=(k == 0), stop=(k == KD - 1))) for jj in range(4) for k in range(KD)],
                        reads=[wmt, "scT"], writes=[pbt])
                    for s_ in range(2):
                        P.I("dve", "tensor_tensor", modF[:, j0:j0 + 4, s_],
                            pb[:, 0:8].rearrange("p (j s) -> p j s", s=2)[:, :, s_], bmF[:, j0:j0 + 4], ALU.add,
                            reads=[pbt, "bmF"], writes=["modF"])
            for base in (KD, 4 * KD):
                P.I("dve", "tensor_scalar", modF[:, base:base + KD, :], modF[:, base:base + KD, :], 1.0, None, ALU.add,
                    reads=["modF"], writes=["modF"])

        def phase_A_block(l, blk, wi, ctx_out):
            kT, V3, wA = T["kT"], T["V3"], T["wA"]
            wkd = wA[:, :, 0:256].rearrange("p k (h c) -> p k h c", h=2)
            wv = wA[:, :, 256:384]
            wft = wA[:, :, 512:1024]
            t0, n, s_ = blk
            nt = n // 128
            hTb, hTtoks = make_hT(blk, (lambda tile: x_src(l, tile)), 0, KD)
            P.D("pool", [(hT_d[:, :, t0:t0 + n], hTb[:, :, 0:n])], key=("hTst", hTtoks[0][0]), reads=hTtoks, writes=[("hT_d", t0)])
            for h in range(2):
                pz, pzt = ps_next()
                mm(pz[:, 0:n], [(wkd[:, k, h, :], hTb[:, k, 0:n]) for k in range(KD)], reads=hTtoks + ["wA"], writes=[pzt])
                rope_norm(pz[:, 0:n], pzt, 1, t0, n, kT[:, h, t0:t0 + n], ("kT", h, t0))
            for ti in range(nt):
                tile = t0 // 128 + ti
                pz, pzt = ps_next()
                mm(pz[:, 0:128], [(hTb[:, k, ti * 128:(ti + 1) * 128], wv[:, k, :]) for k in range(KD)], reads=hTtoks + ["wA"], writes=[pzt])
                pzv = pz[:, 0:128].rearrange("p (h c) -> p h c", h=2)
                P.I("act", "activation", V3[:, tile, :, 0:64], pzv, AF.Copy, reads=[pzt], writes=[("V3a", tile)])
                P.I("pool", "tensor_copy", V3[:, tile, :, 128:192], V3[:, tile, :, 0:64], reads=[("V3a", tile)], writes=[("V3b", tile)])
                if s_ == 0 or ctx_out:
                    pf, pft = ps_next()
                    mm(pf[:], [(hTb[:, k, ti * 128:(ti + 1) * 128], wft[:, k, :]) for k in range(KD)], reads=hTtoks + ["wA"], writes=[pft])
                    fb, fbt = R["tbf"].next()
                    P.I("act", "activation", fb[:], pf[:], AF.Copy, reads=[pft], writes=[fbt])
                    P.D("pool", [(ftok_d[tile], fb[:])], key=fbt, reads=[fbt], writes=[("ftok_d", tile)])

        def attn_prologue(blk):
            wq = T["wA"]
            t0, n, s_ = blk
            hTb, hTtoks = load_hT(blk)
            qs = []
            for c in range(4):
                pz, pzt = ps_next()
                mm(pz[:, 0:n], [(wq[:, k, c * 128:(c + 1) * 128], hTb[:, k, 0:n]) for k in range(KD)], reads=hTtoks + ["wA"], writes=[pzt])
                qT, qTt = R["qT"].next()
                rope_norm(pz[:, 0:n], pzt, 0, t0, n, qT[:, 0:n], qTt)
                qs.append((qT, qTt))
            return qs

        def attn_block(blk, qs, next_blk):
            kT, V3, rsum, bcs, rhl, sAB = T["kT"], T["V3"], T["rsum"], T["bcs"], T["rhl"], T["sAB"]
            t0, n, s_ = blk
            yb, ybt = R["yblk"].next()
            key_tiles = list(range(NT, TT)) + (list(range(NT)) if s_ == 0 else [])
            nk = len(key_tiles)
            poA, poAt, poB, poBt, pbc, pbct = PB[4], "po", PB[5], "pbc", PTF[0], ("pt", 0)
            pending = {}
            next_qs = [None]

            def emit_sT(c, ki):
                qT, qTt = qs[c]
                kv = c // 2
                kt = key_tiles[ki]
                pa, pat = ps_next()
                pb, pbt = ps_next()
                P.M("pe", [("matmul", (pa[:, 0:n], kT[0:64, kv, kt * 128:(kt + 1) * 128], qT[0:64, 0:n]), dict(start=True, stop=True)),
                           ("matmul", (pb[:, 0:n], kT[64:128, kv, kt * 128:(kt + 1) * 128], qT[64:128, 0:n]), dict(start=True, stop=True))],
                    reads=[qTt] + all_k, writes=[pat, pbt])
                pending[(c, ki)] = (pa, pat, pb, pbt)

            def fin_copy(c):
                P.I("act", "activation", sAB[:, 0, 0:n], poA[:, 0:n], AF.Copy, reads=[poAt], writes=[("sAB", 0)])
                P.I("dve", "tensor_copy", sAB[:, 1, 0:n], poB[:, 0:n], reads=[poBt], writes=[("sAB", 1)])
                P.I("dve", "reciprocal", rsum[64:128, 0:n], sAB[64:128, 0, 0:n], reads=[("sAB", 0)], writes=[("rsum", 0)])
                P.I("dve", "reciprocal", rsum[0:64, 0:n], sAB[0:64, 1, 0:n], reads=[("sAB", 1)], writes=[("rsum", 1)])
                P.I("dve", "tensor_copy", rhl[:, 0, 0:n], rsum[:, 0:n], reads=[("rsum", 0), ("rsum", 1)], writes=["rhi"])
                P.I("dve", "tensor_tensor", rhl[:, 1, 0:n], rsum[:, 0:n], rhl[:, 0, 0:n], ALU.subtract,
                    reads=[("rsum", 0), ("rsum", 1), "rhi"], writes=["rlo"])

            def fin_norm(c):
                P.M("pe", [("matmul", (pbc[:, 0:n], selb[:], rhl[:, 0, 0:n]), dict(start=True, stop=False)),
                           ("matmul", (pbc[:, 0:n], selb[:], rhl[:, 1, 0:n]), dict(start=False, stop=True))],
                    reads=["rhi", "rlo", "selb"], writes=[pbct])
                P.I("dve", "tensor_tensor", yb[0:64, c, 0:n], sAB[0:64, 0, 0:n], pbc[0:64, 0:n], ALU.mult,
                    reads=[("sAB", 0), pbct], writes=[(ybt, c, 0)])
                P.I("dve", "tensor_tensor", yb[64:128, c, 0:n], sAB[64:128, 1, 0:n], pbc[64:128, 0:n], ALU.mult,
                    reads=[("sAB", 1), pbct], writes=[(ybt, c, 1)])

            DEFER = 14
            emit_sT(0, 0)
            for c in range(4):
                kv = c // 2
                for ki in range(nk):
                    kt = key_tiles[ki]
                    pa, pat, pb, pbt = pending.pop((c, ki))
                    pTa, pTat = R["pT"].next()
                    pTb, pTbt = R["pT"].next()
                    P.I("act", "activation", pTa[:, 0:n], pa[:, 0:n], AF.Exp, scale=0.125, reads=[pat], writes=[pTat])
                    P.I("act", "activation", pTb[:, 0:n], pb[:, 0:n], AF.Exp, scale=0.125, reads=[pbt], writes=[pTbt])
                    if ki + 1 < nk:
                        emit_sT(c, ki + 1)
                    elif c + 1 < 4:
                        if c + 1 == 2 and next_blk is not None:
                            next_qs[0] = attn_prologue(next_blk)
                        emit_sT(c + 1, 0)
                    P.M("pe", [("matmul", (poA[:, 0:n], V3[:, kt, kv, 0:128], pTa[:, 0:n]), dict(start=(ki == 0), stop=(ki == nk - 1))),
                               ("matmul", (poB[:, 0:n], V3[:, kt, kv, 64:192], pTb[:, 0:n]), dict(start=(ki == 0), stop=(ki == nk - 1)))],
                        reads=[pTat, pTbt, ("V3a", kt), ("V3b", kt), "V3ones"], writes=[poAt, poBt])
                    if c > 0 and ki == min(DEFER, nk - 1):
                        fin_norm(c - 1)
                if c == 3:
                    fin_copy(c)
                    fin_norm(c)
                else:
                    fin_copy(c)
            P.D("pool", [(yb_d[0][:, :, t0:t0 + n], yb[:, :, 0:n])], key=("yst", ybt),
                reads=[(ybt, c, h_) for c in range(4) for h_ in range(2)], writes=[("y_d", 0, t0)])
            return next_qs[0]

        def sg_block(blk):
            wA, wB, sgbias = T["wA"], T["wB"], T["sgbias"]
            wsgv = wA[:, :, 0:512]
            wu_ = wA[:, :, 512:1024]
            wsT = wB
            t0, n, s_ = blk
            nt = n // 128
            hTb, hTtoks = load_hT(blk)
            mx, mxt = R["mx"].next()
            for ti in range(nt):
                pz, pzt = ps_next()
                mm(pz[:], [(hTb[:, k, ti * 128:(ti + 1) * 128], wsgv[:, k, :]) for k in range(KD)], reads=hTtoks + ["wA"], writes=[pzt])
                gv, gvt = R["gv"].next()
                gelu(pz[:], pzt, gv[:], gvt, 512)
                st, stt = R["stt"].next()
                mv, mvt = R["mv"].next()
                sd, sdt = R["sd"].next()
                rs, rst = R["rs"].next()
                P.M("dve", [("bn_stats", (st[:, g, :], gv[:, g * 128:(g + 1) * 128]), {}) for g in range(4)], reads=[gvt], writes=[stt])
                P.M("dve", [("bn_aggr", (mv[:, g, :], st[:, g, :]), {}) for g in range(4)], reads=[stt], writes=[mvt])
                P.I("act", "activation", sd[:], mv[:, :, 1], AF.Sqrt, bias=epsb[:], reads=[mvt, "epsb"], writes=[sdt])
                P.I("dve", "reciprocal", rs[:], sd[:], reads=[sdt], writes=[rst])
                vg, vgt = R["vg"].next()
                for g in range(4):
                    P.I("dve", "tensor_scalar", vg[:, g * 128:(g + 1) * 128], gv[:, g * 128:(g + 1) * 128], mv[:, g, 0:1], rs[:, g:g + 1],
                        ALU.subtract, ALU.mult, reads=[gvt, mvt, rst], writes=[(vgt, g)])
                pm, pmt = ps_next()
                P.M("pe", [("matmul", (pm[:, g * 128:(g + 1) * 128], vg[:, g * 128:(g + 1) * 128], wsT[:, g, :]), dict(start=True, stop=True))
                           for g in range(4)], reads=[(vgt, g) for g in range(4)] + ["wB"], writes=[pmt])
                P.I("dve", "tensor_tensor", mx[:, :, ti * 128:(ti + 1) * 128], pm[:].rearrange("p (g c) -> p g c", g=4),
                    sgbias[:].rearrange("p (g c) -> p g c", g=4), ALU.add, reads=[pmt, "sgbias"], writes=[(mxt, ti)])
            yb, ybt = R["yblk"].next()
            for g in range(4):
                pu, put = ps_next()
                mm(pu[:, 0:n], [(wu_[:, k, g * 128:(g + 1) * 128], hTb[:, k, 0:n]) for k in range(KD)], reads=hTtoks + ["wA"], writes=[put])
                gelu(pu[:, 0:n], put, yb[:, g, 0:n], (ybt, g), n, extra_mul=mx[:, g, 0:n], extra_toks=[(mxt, ti) for ti in range(nt)])
            P.D("pool", [(yb_d[1][:, :, t0:t0 + n], yb[:, :, 0:n])], key=("yst", ybt),
                reads=[(ybt, g) for g in range(4)], writes=[("y_d", 1, t0)])

        def fourier(tile0, ntile, table, nkb, kw, tokbase):
            ftok_sb, Gsb = T["ftok_sb"], T["Gsb"]
            Ltot = ntile * 128
            scale = float(1.0 / np.sqrt(Ltot * 128.0))
            P.D("sp", [(ftok_sb[:, i, :], ftok_d[tile0 + i]) for i in range(ntile)],
                key="ftok_sb", reads=[("ftok_d", tile0 + i) for i in range(ntile)], writes=["ftok_sb"])
            for kb in range(nkb):
                for lc in range(ntile):
                    dt_, dtt = R["dft"].next()
                    P.D("sp", [(dt_[:, :, 0:kw], table[lc, kb])], key=dtt, writes=[dtt])
                    P.M("pe", [("matmul", (PB[g][:, cs_ * 256:cs_ * 256 + kw], ftok_sb[:, lc, g * 128:(g + 1) * 128], dt_[:, cs_, 0:kw]),
                                dict(start=(lc == 0 and cs_ == 0), stop=(lc == ntile - 1), skip_group_check=True)) for g in range(4) for cs_ in range(2)],
                        reads=[dtt, "ftok_sb"], writes=[("ps", g) for g in range(4)])
                for g in range(4):
                    src = PB[g][:].rearrange("p (c k) -> p c k", c=2)[:, :, 0:kw]
                    if g % 2 == 0:
                        P.I("act", "activation", Gsb[:, g, :, 0:kw], src, AF.Copy, reads=[("ps", g)], writes=[("Gsb", g)])
                    else:
                        P.I("dve", "tensor_copy", Gsb[:, g, :, 0:kw], src, reads=[("ps", g)], writes=[("Gsb", g)])
                yf, yft = R["yfr"].next()
                for g in range(4):
                    po_ = PB[4] if g % 2 == 0 else PB[5]
                    pot = "po" if g % 2 == 0 else "pbc"
                    mm(po_[:, 0:kw], [(c128[:, 0, :], Gsb[:, g, 0, 0:kw]), (c128[:, 1, :], Gsb[:, g, 1, 0:kw])],
                       reads=[("Gsb", g), "c128"], writes=[pot])
                    P.I("act", "activation", yf[:, g, 0:kw], po_[:, 0:kw], AF.Copy, scale=scale, reads=[pot], writes=[(yft, g)])
                tb = tokbase + kb * kw
                P.D("pool", [(yb_d[2][:, :, tb:tb + kw], yf[:, :, 0:kw])], key=("yfst", yft),
                    reads=[(yft, g) for g in range(4)], writes=[("y_d", 2, tb)])

        def merge_loads(blk):
            t0, n, s_ = blk
            hTb, hTtoks = load_hT(blk)
            ys = []
            for n_ in range(3):
                yb, ybt = R["ybs%d" % n_].next()
                if n_ == 2:
                    rd = [("y_d", 2, t0 + i * 256) for i in range(n // 256)] if s_ == 0 else [("y_d", 2, t0)]
                else:
                    rd = [("y_d", n_, t0)]
                P.D("sp", [(yb[:, :, 0:n], yb_d[n_][:, :, t0:t0 + n])], key=("ybld", ybt), reads=rd, writes=[ybt])
                ys.append((yb, ybt))
            return hTb, hTtoks, ys

        def merge_block(l, blk, loaded, next_blk):
            wA, wB, wC = T["wA"], T["wB"], T["wC"]
            wgate = wA
            t0, n, s_ = blk
            nt = n // 128
            hTb, hTtoks, ys = loaded
            mT, mTt = R["mT"].next()
            for dc in range(KD):
                acc, acct = R["acc"].next()
                for n_ in range(3):
                    yb, ybt = ys[n_]
                    pp, ppt = ps_next()
                    mm(pp[:, 0:n], [(wB[:, n_, c, dc * 128:(dc + 1) * 128], yb[:, c, 0:n]) for c in range(4)], reads=[ybt, "wB"], writes=[ppt])
                    pg, pgt = ps_next()
                    mm(pg[:, 0:n], [(wgate[:, k, n_ * D + dc * 128:n_ * D + (dc + 1) * 128], hTb[:, k, 0:n]) for k in range(KD)],
                       reads=hTtoks + ["wA"], writes=[pgt])
                    sg, sgt = R["tmpA"].next()
                    P.I("act", "activation", sg[:, 0:n], pg[:, 0:n], AF.Sigmoid, reads=[pgt], writes=[sgt])
                    if n_ == 0:
                        P.I("dve", "tensor_tensor", acc[:, 0:n], sg[:, 0:n], pp[:, 0:n], ALU.mult, reads=[sgt, ppt], writes=[acct])
                    else:
                        P.I("dve", "tensor_tensor", sg[:, 0:n], sg[:, 0:n], pp[:, 0:n], ALU.mult, reads=[sgt, ppt], writes=[sgt])
                        if n_ == 1:
                            P.I("pool", "tensor_tensor", acc[:, 0:n], acc[:, 0:n], sg[:, 0:n], ALU.add, reads=[sgt, acct], writes=[acct])
                        else:
                            P.I("dve", "tensor_tensor", mT[:, dc, 0:n], acc[:, 0:n], sg[:, 0:n], ALU.add, reads=[sgt, acct], writes=[(mTt, dc)])
            nxt = merge_loads(next_blk) if next_blk is not None else None
            for tg in range(0, nt, 2):
                grp = []
                for ti in range(tg, min(tg + 2, nt)):
                    tile = t0 // 128 + ti
                    pys = []
                    for cb in range(NF):
                        py, pyt = ps_next()
                        mm(py[:], [(mT[:, dc, ti * 128:(ti + 1) * 128], wC[:, dc, cb * 512:(cb + 1) * 512]) for dc in range(KD)],
                           reads=[(mTt, dc) for dc in range(KD)] + ["wC"], writes=[pyt])
                        pys.append((py[:], pyt))
                    src, srctok = x_src(l, tile)
                    grp.append(((lambda cb, pys=pys: pys[cb]), x1_d[tile * 128:(tile + 1) * 128, :], ("x1_d", tile), src, srctok))
                post_norm_multi(grp, 0 + s_, 0, 1)
            return nxt

        def router_tile(hTb, hTtoks, ti, lt):
            wr, gates = T["wr"], T["gates"]
            pr, prt = ps_next()
            mm(pr[:, 0:NE], [(hTb[:, k, ti * 128:(ti + 1) * 128], wr[:, k, 0:NE]) for k in range(KD)], reads=hTtoks + ["wr"], writes=[prt])
            lg, lgt = R["tmpA"].next()
            m8, m8t = R["tmpB"].next()
            P.I("dve", "tensor_copy", lg[:, 0:8], pr[:, 0:8], reads=[prt], writes=[lgt])
            P.I("dve", "max", m8[:, 0:8], lg[:, 0:8], reads=[lgt], writes=[m8t])
            P.I("dve", "tensor_tensor", m8[:, 8:9], m8[:, 0:1], m8[:, 1:2], ALU.subtract, reads=[m8t], writes=[(m8t, "d")])
            P.I("act", "activation", m8[:, 9:10], m8[:, 8:9], AF.Sigmoid, reads=[(m8t, "d")], writes=[(m8t, "w1")])
            P.I("act", "activation", m8[:, 10:11], m8[:, 8:9], AF.Sigmoid, scale=-1.0, reads=[(m8t, "d")], writes=[(m8t, "w2")])
            P.I("dve", "tensor_scalar", lg[:, 8:16], lg[:, 0:8], m8[:, 0:1], m8[:, 9:10], ALU.is_equal, ALU.mult,
                reads=[lgt, m8t, (m8t, "w1")], writes=[(lgt, "g1")])
            P.I("dve", "tensor_scalar", lg[:, 16:24], lg[:, 0:8], m8[:, 1:2], m8[:, 10:11], ALU.is_equal, ALU.mult,
                reads=[lgt, m8t, (m8t, "w2")], writes=[(lgt, "g2")])
            P.I("dve", "tensor_tensor", gates[:, lt, :], lg[:, 8:16], lg[:, 16:24], ALU.add,
                reads=[(lgt, "g1"), (lgt, "g2")], writes=[("gates", lt)])

        def ffn(l, ctx_out, last):
            moe = (l % 2 == 1)
            li = l // 2
            if moe:
                nexp, dff = NE, cfg.DFFE
            else:
                nexp, dff = 1, cfg.DFF
            nch = dff // 128
            slabs = [(c0, min(SL, nch - c0)) for c0 in range(0, nch, SL)]
            half_tiles = NT // 2
            bph = half_tiles // 4
            tok_sets = [[(b_ * 512, 512, 0) for b_ in range(h_ * bph, (h_ + 1) * bph)] for h_ in range(2)]
            if ctx_out:
                tok_sets.append([ctx_block])
            fixed = [("yacc", [128, HALF_T, D], F32), ("hT2", [128, KD, HALF_T * 128], BF16),
                     ("gates", [128, HALF_T, 8], F32), ("wr", [128, KD, 8], BF16)]
            for tset in tok_sets:
                base_tok = tset[0][0]
                phase(HT_RINGS + ("tmpA", "tmpB"), fixed)
                keep = ar["off"] if False else None
                yacc, hT2, gates, wr = T["yacc"], T["hT2"], T["gates"], T["wr"]
                fixed_bytes = sum(((int(np.prod(shp[1:])) * (4 if dt == F32 else 2) + 63) // 64 * 64) for _, shp, dt in fixed)
                P.barrier()
                ar["off"] = 0
                for nm, shp, dt in fixed:
                    T[nm] = a_alloc(shp, dt)
                yacc, hT2, gates, wr = T["yacc"], T["hT2"], T["gates"], T["wr"]
                for k_ in list(R.keys()):
                    del R[k_]
                for nm in HT_RINGS + ("tmpA", "tmpB"):
                    make_ring(nm)
                if moe:
                    wload(wr[:, :, 0:NE], "wr", router[li].rearrange("(k p) e -> p k e", p=128), key="wr")
                for blk in tset:
                    t0, n, s_ = blk
                    nt = n // 128
                    hTb, hTtoks = make_hT(blk, (lambda tile: (x1_d[tile * 128:(tile + 1) * 128, :], ("x1_d", tile))), 3 * KD, 4 * KD)
                    off = t0 - base_tok
                    P.I("pool", "tensor_copy", hT2[:, :, off:off + n], hTb[:, :, 0:n], reads=hTtoks, writes=[("hT2", t0)])
                    if moe:
                        for ti in range(nt):
                            router_tile(hTb, hTtoks, ti, off // 128 + ti)
                P.barrier()
                ar["off"] = fixed_bytes
                for k_ in list(R.keys()):
                    del R[k_]
                for nm in ("wgu", "wd", "actT", "tmpC"):
                    make_ring(nm)
                first = True
                for e_ in range(nexp):
                    if moe:
                        wg_, wu2, wd2 = exp_wg[li, e_], exp_wu[li, e_], exp_wd[li, e_]
                    else:
                        wg_, wu2, wd2 = ffn_wg[li], ffn_wu[li], ffn_wd[li]
                    wgv = wg_.rearrange("(k p) f -> p k f", p=128)
                    wuv = wu2.rearrange("(k p) f -> p k f", p=128)
                    wdv = wd2.rearrange("(c p) d -> p c d", p=128)
                    for (c0, ncs) in slabs:
                        wgu, wgut = R["wgu"].next()
                        wd_, wdt = R["wd"].next()
                        P.D("pool", [(wgu[:, :, 0, 0:ncs * 128], wgv[:, :, c0 * 128:(c0 + ncs) * 128]),
                                     (wgu[:, :, 1, 0:ncs * 128], wuv[:, :, c0 * 128:(c0 + ncs) * 128])], key=wgut, writes=[wgut])
                        P.D("pool", [(wd_[:, 0:ncs, :], wdv[:, c0:c0 + ncs, :])], key=wdt, writes=[wdt])
                        for blk in tset:
                            t0, n, s_ = blk
                            nt = n // 128
                            off = t0 - base_tok
                            aT, aTt = R["actT"].next()
                            for ch in range(ncs):
                                pg, pgt = ps_next()
                                pu, put = ps_next()
                                mm(pg[:, 0:n], [(wgu[:, k, 0, ch * 128:(ch + 1) * 128], hT2[:, k, off:off + n]) for k in range(KD)],
                                   reads=[wgut, ("hT2", t0)], writes=[pgt])
                                mm(pu[:, 0:n], [(wgu[:, k, 1, ch * 128:(ch + 1) * 128], hT2[:, k, off:off + n]) for k in range(KD)],
                                   reads=[wgut, ("hT2", t0)], writes=[put])
                                sg, sgt = R["tmpC"].next()
                                P.I("act", "activation", sg[:, 0:n], pg[:, 0:n], AF.Silu, reads=[pgt], writes=[sgt])
                                P.I("dve", "tensor_tensor", aT[:, ch, 0:n], sg[:, 0:n], pu[:, 0:n], ALU.mult, reads=[sgt, put], writes=[(aTt, ch)])
                            for ti in range(nt):
                                lt = off // 128 + ti
                                for cb in range(NF):
                                    py, pyt = ps_next()
                                    mm(py[:], [(aT[:, ch, ti * 128:(ti + 1) * 128], wd_[:, ch, cb * 512:(cb + 1) * 512]) for ch in range(ncs)],
                                       reads=[(aTt, ch) for ch in range(ncs)] + [wdt], writes=[pyt])
                                    ya = yacc[:, lt, cb * 512:(cb + 1) * 512]
                                    yat = ("yacc", lt, cb)
                                    if moe:
                                        gsc = gates[:, lt, e_:e_ + 1]
                                        if first:
                                            P.I("dve", "tensor_scalar", ya, py[:], gsc, None, ALU.mult, reads=[pyt, ("gates", lt)], writes=[yat])
                                        else:
                                            P.I("dve", "scalar_tensor_tensor", ya, py[:], gsc, ya, ALU.mult, ALU.add,
                                                reads=[pyt, ("gates", lt), yat], writes=[yat])
                                    else:
                                        if first:
                                            P.I("act", "activation", ya, py[:], AF.Copy, reads=[pyt], writes=[yat])
                                        else:
                                            P.I("dve", "tensor_tensor", ya, ya, py[:], ALU.add, reads=[pyt, yat], writes=[yat])
                        first = False
                P.barrier()
                ar["off"] = fixed_bytes
                for k_ in list(R.keys()):
                    del R[k_]
                for nm in (("xt", 4), ("r", 4), ("xo", 4)) + LN_RINGS:
                    make_ring(nm)
                for blk in tset:
                    t0, n, s_ = blk
                    if last and s_ == 1:
                        continue
                    grp = []
                    for ti in range(n // 128):
                        tile = t0 // 128 + ti
                        lt = (t0 - base_tok) // 128 + ti
                        if last:
                            dst, dtok = out_d[tile * 128:(tile + 1) * 128, :], ("out", tile)
                        else:
                            dst, dtok = x2_d[l][tile * 128:(tile + 1) * 128, :], ("x2_d", l, tile)
                        grp.append(((lambda cb, lt=lt: (yacc[:, lt, cb * 512:(cb + 1) * 512], ("yacc", lt, cb))),
                                    dst, dtok, x1_d[tile * 128:(tile + 1) * 128, :], ("x1_d", tile)))
                    post_norm_multi(grp, 2 + s_, 2, 3)

        KV_T = [("kT", [128, 2, TOT], BF16), ("V3", [128, TT, 2, 192], BF16)]
        for l in range(cfg.EMIT_LAYERS):
            last = (l == L - 1)
            ctx_out = not last
            phase(("wm",), [("scB", [128, 2, KD, 128], BF16), ("bmbc", [128, 2, D], F32)])
            modulation(l)
            if cfg.STOP == "mod":
                break
            wi = w_in[l].rearrange("(k p) n -> p k n", p=128)
            phase(HT_RINGS + ("tmpA", "tmpB", "tmpC", "tbf", "tbf2", "rcs"), KV_T + [("wA", [128, KD, 1024], BF16)])
            wA = T["wA"]
            P.I("pool", "memset", T["V3"][:, :, :, 64:128], 1.0, writes=["V3ones"])
            for h in range(2):
                for dup in range(2):
                    c0 = h * 128 + dup * 64
                    wload(wA[:, :, c0:c0 + 64], "wA", wi[:, :, cfg.OFF_K + h * 64:cfg.OFF_K + h * 64 + 64], key=("wA", h, dup))
            wload(wA[:, :, 256:384], "wA", wi[:, :, cfg.OFF_V:cfg.OFF_V + 128], key=("wA", 9))
            wload(wA[:, :, 512:1024], "wA", wi[:, :, cfg.OFF_FT:cfg.OFF_FT + 512], key=("wA", 10))
            for blk in lat_blocks + [ctx_block]:
                phase_A_block(l, blk, wi, ctx_out)
            q_blocks = lat_blocks + ([ctx_block] if ctx_out else [])
            if cfg.STOP == "A":
                break
            phase(("hTb", "yblk", "qT", "pT", "tmpA", "tmpB", "tmpC", "tbf", "tbf2", "rcs"),
                  KV_T + [("wA", [128, KD, 512], BF16), ("rsum", [128, 512], F32), ("bcs", [128, 512], F32), ("rhl", [128, 2, 512], BF16), ("sAB", [128, 2, 512], F32)])
            wload(T["wA"][:], "wA", wi[:, :, cfg.OFF_Q:cfg.OFF_Q + 512], key=("wA", 11))
            qs_cur = attn_prologue(q_blocks[0])
            for bi, blk in enumerate(q_blocks):
                nb_ = q_blocks[bi + 1] if bi + 1 < len(q_blocks) else None
                qs_cur = attn_block(blk, qs_cur, nb_)
            if cfg.STOP == "B1":
                break
            phase(("hTb", "yblk", "mx", "gv", "vg", "tmpA", "tmpB") + LN_RINGS,
                  [("wA", [128, KD, 1024], BF16), ("wB", [128, 4, 128], BF16), ("sgbias", [128, 512], F32)])
            wload(T["wA"][:, :, 0:512], "wA", wi[:, :, cfg.OFF_SGV:cfg.OFF_SGV + 512], key=("wA", 12))
            wload(T["wA"][:, :, 512:1024], "wA", wi[:, :, cfg.OFF_U:cfg.OFF_U + 512], key=("wA", 13))
            wload(T["wB"][:], "wB", sg_wT[l], key=("wB", 0))
            P.D("sp", [(T["sgbias"][:], sg_b[l:l + 1, :].broadcast_to([128, 512]))], key="sgbias", writes=["sgbias"])
            for blk in q_blocks:
                sg_block(blk)
            if cfg.STOP == "B2":
                break
            phase(("dft", "yfr"), [("ftok_sb", [128, NT, 512], BF16), ("Gsb", [128, 4, 2, 256], BF16)])
            fourier(0, NT, dftL_d, cfg.NKB, 256, 0)
            if ctx_out:
                fourier(NT, NCT, dftC_d, 1, CTX, S)
            if cfg.STOP == "B3":
                break
            phase(("hTb", "ybs0", "ybs1", "ybs2", "mT", "acc", "tmpA", "xt", "r", "xo") + LN_RINGS,
                  [("wA", [128, KD, 3 * D], BF16), ("wB", [128, 3, 4, D], BF16), ("wC", [128, KD, D], BF16)])
            wload(T["wA"][:], "wA", wi[:, :, cfg.OFF_GATE:cfg.OFF_GATE + 3 * D], key=("wA", 14))
            wload(T["wB"][:], "wB", w_branch[l].rearrange("n (c p) d -> p n c d", p=128), key=("wB", 1))
            wload(T["wC"][:], "wC", w_out[l].rearrange("(k p) d -> p k d", p=128), key=("wC", 0))
            ld_cur = merge_loads(q_blocks[0])
            for bi, blk in enumerate(q_blocks):
                nb_ = q_blocks[bi + 1] if bi + 1 < len(q_blocks) else None
                ld_cur = merge_block(l, blk, ld_cur, nb_)
            if cfg.STOP == "B4":
                break
            ffn(l, ctx_out, last)

        if cfg.MAXOPS is not None:
            print("total ops", len(P.ops))
            P.ops = P.ops[:cfg.MAXOPS]
            print("last op:", P.ops[-1].eng, P.ops[-1].reads, P.ops[-1].writes)
        stats = P.emit(final_waits=[("xo", i) for i in range(4)] if (cfg.STOP is None and cfg.MAXOPS is None) else [])
    return nc, stats


def make_in_maps(cfg, inputs, nb):
    D, KD = cfg.D, cfg.KD
    L = cfg.DEPTH
    consts = make_consts(cfg)
    f = lambda a: np.ascontiguousarray(np.asarray(a, dtype=np.float32))
    shared = dict(consts)
    shared["w_mod"] = f(inputs["w_mod"])
    bm = f(inputs["b_mod"])
    shared["b_mod"] = bm
    shared["b_modF"] = np.ascontiguousarray(bm.reshape(L, 6 * KD, 128).transpose(0, 2, 1))
    shared["w_in"] = f(inputs["w_in"])
    qn = np.tile(f(inputs["q_norm"]), (1, 2))
    kn = np.tile(f(inputs["k_norm"]), (1, 2))
    shared["qk_norm"] = np.ascontiguousarray(np.stack([qn, kn], axis=-1))
    shared["sg_wT"] = np.ascontiguousarray(f(inputs["sg_w"]).transpose(0, 3, 1, 2))
    shared["sg_b"] = np.ascontiguousarray(f(inputs["sg_b"]).reshape(L, 512))
    shared["w_branch"] = f(inputs["w_branch"])
    shared["w_out"] = f(inputs["w_out"])
    shared["ln_gb"] = np.ascontiguousarray(np.stack([f(inputs["ln1_g"]), f(inputs["ln1_b"]), f(inputs["ln2_g"]), f(inputs["ln2_b"])], axis=1))
    shared["ffn_w_gate"] = f(inputs["ffn_w_gate"])
    shared["ffn_w_up"] = f(inputs["ffn_w_up"])
    shared["ffn_w_down"] = f(inputs["ffn_w_down"])
    if L // 2:
        shared["router"] = f(inputs["router"])
        shared["exp_w_gate"] = f(inputs["exp_w_gate"])
        shared["exp_w_up"] = f(inputs["exp_w_up"])
        shared["exp_w_down"] = f(inputs["exp_w_down"])
    x = f(inputs["x"])
    ctx = f(inputs["ctx"])
    c = f(inputs["c"])
    cc = f(inputs["c_ctx"])
    maps = []
    for b in range(nb):
        m = dict(shared)
        m["x"] = x[b]
        m["ctx"] = ctx[b]
        cv = np.stack([c[b].reshape(KD, 128).T, cc.reshape(KD, 128).T], axis=-1)
        m["cvecs"] = np.ascontiguousarray(cv)
        maps.append(m)
    return maps


_CACHE = {}


def kernel(**inputs):
    cfg = FULL
    nb = inputs["x"].shape[0]
    if "nc" not in _CACHE:
        _CACHE["nc"] = build_program(cfg)[0]
    nc = _CACHE["nc"]
    maps = make_in_maps(cfg, inputs, nb)
    res = run_bass_kernel_spmd(nc, maps, core_ids=list(range(nb)))
    out = np.stack([np.asarray(r["out"], dtype=np.float32) for r in res.results], axis=0)
    return out
```

```python
import contextlib
import numpy as np
import ml_dtypes
import concourse.bass as bass
import concourse.mybir as mybir
from concourse.bass_utils import run_bass_kernel_spmd

F32 = mybir.dt.float32
BF16 = mybir.dt.bfloat16
ALU = mybir.AluOpType
AF = mybir.ActivationFunctionType

ENGS = ("pe", "act", "dve", "pool", "sp")


class Op:
    __slots__ = ("eng", "fn", "reads", "writes", "dma", "ndma", "key", "deps",
                 "needs_inc", "sem", "val", "waits", "extra")

    def __init__(self, eng, fn, reads, writes, dma=False, ndma=1, key=None):
        self.eng = eng
        self.fn = fn
        self.reads = tuple(reads)
        self.writes = tuple(writes)
        self.dma = dma
        self.ndma = ndma
        self.key = key
        self.deps = ()
        self.needs_inc = False
        self.sem = None
        self.val = 0
        self.waits = ()
        self.extra = ()


def is_psum(t):
    return t in ("po", "pbc") or (isinstance(t, tuple) and len(t) == 2 and t[0] in ("ps", "pt"))


class Prog:
    def __init__(self, nc):
        self.nc = nc
        self.ops = []
        self.last_eng = {}
        self.last_key = {}
        self.bar = ()
        self.bar_pending = set()

    def barrier(self):
        self.bar = tuple(self.last_eng.values()) + tuple(self.last_key.values())
        self.bar_pending = set(ENGS)

    def _track(self, o):
        i = len(self.ops)
        if o.eng in self.bar_pending:
            o.extra = self.bar
            self.bar_pending.discard(o.eng)
        self.ops.append(o)
        if o.dma:
            self.last_key[o.key] = i
        else:
            self.last_eng[o.eng] = i

    def I(self, eng, method, *args, reads=(), writes=(), **kw):
        return self.M(eng, [(method, args, kw)], reads, writes)

    def M(self, eng, insts, reads=(), writes=()):
        insts = list(insts)

        def fn(e):
            r = None
            for (m, a, kw) in insts:
                r = getattr(e, m)(*a, **kw)
            return r
        o = Op(eng, fn, reads, writes)
        self._track(o)
        return o

    def D(self, eng, pairs, key, reads=(), writes=()):
        pairs = list(pairs)

        def fn(e):
            return [e.dma_start(out=o_, in_=i_) for (o_, i_) in pairs]
        o = Op(eng, fn, reads, tuple(writes) + (("dmakey", key),), dma=True, ndma=len(pairs), key=key)
        self._track(o)
        return o

    def analyze(self):
        last_w = {}
        readers = {}
        ops = self.ops
        for i, o in enumerate(ops):
            deps = set()
            for t in o.reads:
                w = last_w.get(t)
                if w is not None:
                    deps.add(w)
                if is_psum(t):
                    for r in readers.get(t, ()):
                        if ops[r].eng != o.eng:
                            deps.add(r)
            for t in o.writes:
                w = last_w.get(t)
                if w is not None:
                    deps.add(w)
                for r in readers.get(t, ()):
                    deps.add(r)
            deps.update(o.extra)
            deps.discard(i)
            keep = []
            for j in deps:
                d = ops[j]
                if d.dma:
                    keep.append(j)
                    continue
                if d.eng == o.eng and not o.dma:
                    if o.eng == "pe":
                        continue
                keep.append(j)
            o.deps = keep
            for j in keep:
                ops[j].needs_inc = True
            for t in o.reads:
                readers.setdefault(t, []).append(i)
            for t in o.writes:
                last_w[t] = i
                readers[t] = []

    def emit(self, final_waits=()):
        nc = self.nc
        self.analyze()
        ops = self.ops
        keys = []
        seen = set()
        for o in ops:
            if o.dma and o.key not in seen:
                seen.add(o.key)
                keys.append(o.key)
        with contextlib.ExitStack() as es:
            esem = {e: es.enter_context(nc.semaphore("s_" + e)) for e in ENGS}
            ksem = {k: es.enter_context(nc.semaphore("k%d" % n)) for n, k in enumerate(keys)}
            cnt = {e: 0 for e in ENGS}
            kcnt = {k: 0 for k in keys}
            for o in ops:
                if o.dma:
                    kcnt[o.key] += 16 * o.ndma
                    o.sem = ksem[o.key]
                    o.val = kcnt[o.key]
                elif o.needs_inc:
                    cnt[o.eng] += 1
                    o.sem = esem[o.eng]
                    o.val = cnt[o.eng]
            known = {e: {} for e in ENGS}
            for o in ops:
                w = {}
                for j in o.deps:
                    d = ops[j]
                    sid = id(d.sem)
                    if sid not in w or w[sid][1] < d.val:
                        w[sid] = (d.sem, d.val)
                kn = known[o.eng]
                ws = []
                for sid, (s, v) in w.items():
                    if kn.get(sid, 0) >= v:
                        continue
                    kn[sid] = v
                    ws.append((s, v))
                o.waits = ws
            per_eng = {e: [o for o in ops if o.eng == e] for e in ENGS}
            last_dma = {}
            for o in ops:
                if o.dma:
                    last_dma[o.key] = o
            block = es.enter_context(nc.Block())

            def run(eng_name, eng):
                for o in per_eng[eng_name]:
                    for s, v in o.waits:
                        eng.wait_ge(s, v)
                    r = o.fn(eng)
                    if o.dma:
                        rs = r if isinstance(r, (list, tuple)) else [r]
                        assert len(rs) == o.ndma, (len(rs), o.ndma)
                        for ins in rs:
                            ins.then_inc(o.sem, 16)
                    elif o.needs_inc:
                        r.then_inc(o.sem, 1)
                if eng_name == "sp":
                    for k in (final_waits if final_waits else list(last_dma.keys())):
                        if k in last_dma:
                            o = last_dma[k]
                            eng.wait_ge(o.sem, o.val)

            @block.tensor
            def _(e):
                run("pe", e)

            @block.scalar
            def _(e):
                run("act", e)

            @block.vector
            def _(e):
                run("dve", e)

            @block.gpsimd
            def _(e):
                run("pool", e)

            @block.sync
            def _(e):
                run("sp", e)
        return {e: len(per_eng[e]) for e in ENGS}, len(keys)


class Cfg:
    def __init__(self, D=1024, S=4096, CTX=256, DFF=2816, NE=8, DFFE=3584, DEPTH=2, GRID_W=64, SL=4):
        self.D, self.S, self.CTX, self.DFF, self.NE, self.DFFE = D, S, CTX, DFF, NE, DFFE
        self.DEPTH, self.GRID_W, self.SL = DEPTH, GRID_W, SL
        self.KD = D // 128
        self.NT = S // 128
        self.NCT = CTX // 128
        self.TOT = S + CTX
        self.TT = self.NT + self.NCT
        self.OFF_Q, self.OFF_K, self.OFF_V, self.OFF_U = 0, 512, 640, 768
        self.OFF_SGV, self.OFF_FT, self.OFF_GATE = 1280, 1792, 2304
        self.INW = 2304 + 3 * D
        self.ALPHA = float((2 * DEPTH) ** 0.25)
        self.BW = 512
        self.NKB = S // 256
        self.ARENA_KB = 166
        self.EMIT_LAYERS = DEPTH
        self.STOP = None
        self.MAXOPS = None


FULL = Cfg()


def make_consts(cfg):
    bf = ml_dtypes.bfloat16
    S, CTX, TOT, GW = cfg.S, cfg.CTX, cfg.TOT, cfg.GRID_W
    c = {}
    c["identb"] = np.eye(128, dtype=np.float32).astype(bf)
    P = np.zeros((128, 128), np.float32)
    for m in range(128):
        if (m % 32) < 16:
            P[m + 16, m] = -1.0
        else:
            P[m - 16, m] = 1.0
    c["rotP"] = P.astype(bf)
    B = np.zeros((128, 128), np.float32)
    B[:64, :64] = 1.0
    B[64:, 64:] = 1.0
    c["blockones"] = B.astype(bf)
    sel = np.zeros((128, 128), np.float32)
    sel[64, 0:64] = 1.0
    sel[0, 64:128] = 1.0
    c["onesb"] = sel.astype(bf)
    t = np.arange(S)
    row = (t // GW).astype(np.float64)
    col = (t % GW).astype(np.float64)
    i = np.arange(128) % 64
    j = i % 16
    inv = 10000.0 ** (-(j.astype(np.float64)) / 16.0)
    pos = np.where((i < 32)[:, None], row[None, :], col[None, :])
    ang = pos * inv[:, None]
    cs = np.zeros((2, 128, TOT), np.float32)
    cs[0, :, :S] = np.cos(ang)
    cs[1, :, :S] = np.sin(ang)
    cs[0, :, S:] = 1.0
    c["ropecs"] = cs

    def dft_table(L, nkb, kw):
        l = np.arange(L, dtype=np.int64)
        k = np.arange(L, dtype=np.int64)
        ph = (np.outer(l, k) % L).astype(np.float64) * (2.0 * np.pi / L)
        C = np.cos(ph).astype(np.float32)
        Sn = np.sin(ph).astype(np.float32)
        tab = np.stack([C, Sn], axis=1)
        tab = tab.reshape(L // 128, 128, 2, nkb, kw).transpose(0, 3, 1, 2, 4)
        return np.ascontiguousarray(tab).astype(bf)

    c["dftL"] = dft_table(S, cfg.NKB, 256)
    c["dftC"] = dft_table(CTX, 1, CTX)
    d = np.arange(128, dtype=np.int64)
    ph = (np.outer(d, d) % 128).astype(np.float64) * (2.0 * np.pi / 128)
    c["c128"] = np.stack([np.cos(ph), -np.sin(ph)], axis=1).astype(np.float32).astype(bf)
    return c


def build_program(cfg, debug_outs=()):
    nc = bass.Bass("TRN2", target_bir_lowering=False)
    D, S, CTX, TOT, KD, NT, NCT, TT = cfg.D, cfg.S, cfg.CTX, cfg.TOT, cfg.KD, cfg.NT, cfg.NCT, cfg.TT
    L, NE, INW = cfg.DEPTH, cfg.NE, cfg.INW
    ND = (L + 1) // 2
    NM = L // 2

    def din(name, shape, dt=F32):
        return nc.dram_tensor(name, list(shape), dt, kind="ExternalInput").ap()

    def dscr(name, shape, dt):
        kind = "ExternalOutput" if name in debug_outs else "Internal"
        return nc.dram_tensor(name, list(shape), dt, kind=kind).ap()

    x_in = din("x", [S, D])
    ctx_in = din("ctx", [CTX, D])
    cvec = din("cvecs", [128, KD, 2])
    w_mod = din("w_mod", [L, D, 6 * D])
    b_modF = din("b_modF", [L, 128, 6 * KD])
    b_mod = din("b_mod", [L, 6 * D])
    w_in = din("w_in", [L, D, INW])
    qk_norm = din("qk_norm", [L, 128, 2])
    sg_wT = din("sg_wT", [L, 128, 4, 128])
    sg_b = din("sg_b", [L, 512])
    w_branch = din("w_branch", [L, 3, 512, D])
    w_out = din("w_out", [L, D, D])
    ln_gb = din("ln_gb", [L, 4, D])
    ffn_wg = din("ffn_w_gate", [ND, D, cfg.DFF])
    ffn_wu = din("ffn_w_up", [ND, D, cfg.DFF])
    ffn_wd = din("ffn_w_down", [ND, cfg.DFF, D])
    if NM:
        router = din("router", [NM, D, NE])
        exp_wg = din("exp_w_gate", [NM, NE, D, cfg.DFFE])
        exp_wu = din("exp_w_up", [NM, NE, D, cfg.DFFE])
        exp_wd = din("exp_w_down", [NM, NE, cfg.DFFE, D])
    identb_d = din("identb", [128, 128], BF16)
    rotP_d = din("rotP", [128, 128], BF16)
    blockones_d = din("blockones", [128, 128], BF16)
    onesb_d = din("onesb", [128, 128], BF16)
    ropecs_d = din("ropecs", [2, 128, TOT])
    dftL_d = din("dftL", [NT, cfg.NKB, 128, 2, 256], BF16)
    dftC_d = din("dftC", [NCT, 1, 128, 2, CTX], BF16)
    c128_d = din("c128", [128, 2, 128], BF16)
    out_d = nc.dram_tensor("out", [S, D], F32, kind="ExternalOutput").ap()

    hT_d = dscr("hT_d", [128, KD, TOT], BF16)
    ftok_d = dscr("ftok_d", [TT, 128, 512], BF16)
    yb_d = [dscr("y%d_d" % n, [128, 4, TOT], BF16) for n in range(3)]
    x1_d = dscr("x1_d", [TOT, D], F32)
    x2_d = [dscr("x2_d%d" % l, [TOT, D], F32) for l in range(max(L - 1, 1))]

    P = Prog(nc)
    es = contextlib.ExitStack()
    with es:
        def sb(name, shape, dt):
            return es.enter_context(nc.sbuf_tensor("s_" + name, list(shape), dt))

        PB = [es.enter_context(nc.psum_tensor("pb%d" % i, [128, 512], F32)) for i in range(6)]
        PT = [es.enter_context(nc.psum_tensor("pt%d" % i, [128, 1024], BF16)) for i in range(2)]
        PTF = [PT[i].bitcast(F32) for i in range(2)]
        ring_state = {"i": 0}

        def ps_next():
            i = ring_state["i"]
            ring_state["i"] = (i + 1) % 4
            return PB[i], ("ps", i)

        pt_state = {"i": 0}

        def pt_next():
            i = pt_state["i"]
            pt_state["i"] = 1 - i
            return PT[i][:, 0:512], ("pt", i)

        class Ring:
            def __init__(self, name, n, shape, dt):
                self.t = [sb("%s%d" % (name, i), shape, dt) for i in range(n)]
                self.name = name
                self.n = n
                self.i = 0

            def next(self):
                i = self.i
                self.i = (i + 1) % self.n
                return self.t[i], (self.name, i)

        identb = sb("identb", [128, 128], BF16)
        rotP = sb("rotP", [128, 128], BF16)
        blockones = sb("blockones", [128, 128], BF16)
        selb = sb("onesb", [128, 128], BF16)
        c128 = sb("c128", [128, 2, 128], BF16)
        epsb = sb("epsb", [128, 1], F32)
        zeros = sb("zeros", [128, 128], F32)
        for nm, t, d in (("identb", identb, identb_d), ("rotP", rotP, rotP_d), ("blockones", blockones, blockones_d),
                         ("selb", selb, onesb_d), ("c128", c128, c128_d)):
            P.D("sp", [(t[:], d)], key=nm, writes=[nm])
        P.I("pool", "memset", epsb[:], 1e-6, writes=["epsb"])
        P.I("pool", "memset", zeros[:], 0.0, writes=["zeros"])

        cv32 = sb("cv32", [128, KD, 2], F32)
        scT = sb("scT", [128, KD, 2], BF16)
        sc32 = sb("sc32", [128, KD, 2], F32)
        modF = sb("modF", [128, 6 * KD, 2], F32)
        bmF = sb("bmF", [128, 6 * KD], F32)
        bcg = sb("bcg", [128, 4, D], F32)
        lngb = sb("lngb", [128, 4, D], F32)
        qkn = sb("qkn", [128, 2], F32)

        P.D("sp", [(cv32[:], cvec)], key="cv32", writes=["cv32"])
        P.I("act", "activation", sc32[:], cv32[:], AF.Silu, reads=["cv32"], writes=["sc32"])
        P.I("dve", "tensor_copy", scT[:], sc32[:], reads=["sc32"], writes=["scT"])

        ARENA_BYTES = cfg.ARENA_KB * 1024
        arena_f = sb("arena", [128, ARENA_BYTES // 4], F32)
        arena_b = arena_f.bitcast(BF16)
        ar = {"off": 0}
        R = {}
        T = {}

        def a_alloc(shape, dt):
            n = 1
            for d_ in shape[1:]:
                n *= d_
            esz = 4 if dt == F32 else 2
            off = ar["off"]
            nbytes = (n * esz + 63) // 64 * 64
            assert off + nbytes <= ARENA_BYTES, ("arena overflow", off, nbytes, ARENA_BYTES)
            ar["off"] = off + nbytes
            base = arena_f if dt == F32 else arena_b
            v = base[:, off // esz:off // esz + n]
            if len(shape) == 3:
                v = v.rearrange("p (a b) -> p a b", a=shape[1])
            elif len(shape) == 4:
                v = v.rearrange("p (a b c) -> p a b c", a=shape[1], b=shape[2])
            return v

        class Ring:
            def __init__(self, name, n, shape, dt):
                self.t = [a_alloc(shape, dt) for i in range(n)]
                self.name = name
                self.n = n
                self.i = 0

            def next(self):
                i = self.i
                self.i = (i + 1) % self.n
                return self.t[i], (self.name, i)

        SPEC = {
            "wm": (2, [128, KD, 512], BF16), "xt": (2, [128, D], F32), "xn4": (2, [128, 4, D], BF16),
            "hTb": (2, [128, KD, 512], BF16), "stt": (4, [128, 8, 6], F32), "mv": (4, [128, 4, 2], F32),
            "sd": (4, [128, 4], F32), "rs": (4, [128, 4], F32),
            "tmpA": (2, [128, 512], F32), "tmpB": (2, [128, 512], F32), "tmpC": (2, [128, 512], F32),
            "tbf": (2, [128, 512], BF16), "tbf2": (2, [128, 512], BF16), "rcs": (2, [128, 2, 512], F32),
            "r": (2, [128, D], F32), "xo": (2, [128, D], F32),
            "wgu": (2, [128, KD, 2, cfg.SL * 128], BF16), "wd": (2, [128, cfg.SL, D], BF16), "actT": (2, [128, cfg.SL, 512], BF16),
            "yblk": (2, [128, 4, 512], BF16), "qT": (8, [128, 512], BF16), "pT": (4, [128, 512], BF16),
            "mx": (2, [128, 4, 512], F32), "gv": (2, [128, 512], F32), "vg": (2, [128, 512], BF16),
            "dft": (4, [128, 2, 256], BF16), "yfr": (2, [128, 4, 256], BF16),
            "mT": (1, [128, KD, 512], BF16), "ybs0": (1, [128, 4, 512], BF16), "ybs1": (1, [128, 4, 512], BF16),
            "ybs2": (1, [128, 4, 512], BF16), "acc": (2, [128, 512], F32),
        }

        def make_ring(nm):
            cnt = None
            if isinstance(nm, tuple):
                nm, cnt = nm
            n_, shp, dt = SPEC[nm]
            R[nm] = Ring(nm, cnt or n_, shp, dt)

        def phase(rings=(), tensors=(), keep=0):
            P.barrier()
            ar["off"] = keep
            for k_ in list(R.keys()):
                del R[k_]
            for nm, shp, dt in tensors:
                T[nm] = a_alloc(shp, dt)
            for nm in rings:
                make_ring(nm)

        LN_RINGS = ("stt", "mv", "sd", "rs")
        HT_RINGS = (("xt", 4), "xn4", "hTb") + LN_RINGS

        NF = D // 512

        def mm(out_ap, pairs, reads, writes):
            pairs = list(pairs)
            n_ = len(pairs)
            P.M("pe", [("matmul", (out_ap, a, b), dict(start=(i == 0), stop=(i == n_ - 1))) for i, (a, b) in enumerate(pairs)],
                reads, writes)

        def ln_multi(items):
            bufs = []
            for it in items:
                bufs.append((R["stt"].next(), R["mv"].next(), R["sd"].next(), R["rs"].next()))
            for (src_ap, src_tok, dst_ap, dst_tok), ((st, stt), (mv, mvt), (sd, sdt), (rs, rst)) in zip(items, bufs):
                P.M("dve", [("bn_stats", (st[:, f, :], src_ap[:, f * 512:(f + 1) * 512]), {}) for f in range(NF)], reads=[src_tok], writes=[stt])
            for (src_ap, src_tok, dst_ap, dst_tok), ((st, stt), (mv, mvt), (sd, sdt), (rs, rst)) in zip(items, bufs):
                P.I("dve", "bn_aggr", mv[:, 0, :], st[:, 0:NF, :].rearrange("p g s -> p (g s)"), reads=[stt], writes=[mvt])
            for (src_ap, src_tok, dst_ap, dst_tok), ((st, stt), (mv, mvt), (sd, sdt), (rs, rst)) in zip(items, bufs):
                P.I("act", "activation", sd[:, 0:1], mv[:, 0, 1:2], AF.Sqrt, bias=epsb[:], reads=[mvt, "epsb"], writes=[sdt])
            for (src_ap, src_tok, dst_ap, dst_tok), ((st, stt), (mv, mvt), (sd, sdt), (rs, rst)) in zip(items, bufs):
                P.I("dve", "reciprocal", rs[:, 0:1], sd[:, 0:1], reads=[sdt], writes=[rst])
            for (src_ap, src_tok, dst_ap, dst_tok), ((st, stt), (mv, mvt), (sd, sdt), (rs, rst)) in zip(items, bufs):
                P.I("dve", "tensor_scalar", dst_ap, src_ap, mv[:, 0, 0:1], rs[:, 0:1], ALU.subtract, ALU.mult,
                    reads=[src_tok, mvt, rst], writes=[dst_tok])

        def x_src(l, tile):
            if l == 0:
                if tile < NT:
                    return x_in[tile * 128:(tile + 1) * 128, :], None
                return ctx_in[(tile - NT) * 128:(tile - NT + 1) * 128, :], None
            return x2_d[l - 1][tile * 128:(tile + 1) * 128, :], ("x2_d", l - 1, tile)

        def make_hT(blk, src_fn, shift_j, scale_j):
            t0, n, s_ = blk
            nt = n // 128
            xn, xnt = R["xn4"].next()
            items = []
            for ti in range(nt):
                tile = t0 // 128 + ti
                src, srctok = src_fn(tile)
                xt, xtt = R["xt"].next()
                P.D("sp", [(xt[:], src)], key=xtt, reads=[srctok] if srctok else [], writes=[xtt])
                items.append((xt[:], xtt, xn[:, ti, :], (xnt, ti)))
            ln_multi(items)
            hTb, hTt = R["hTb"].next()
            for k in range(KD):
                pt, ptt = pt_next()
                P.M("pe", [("transpose", (pt[:, ti * 128:(ti + 1) * 128], xn[:, ti, k * 128:(k + 1) * 128], identb[:]), {}) for ti in range(nt)],
                    reads=[(xnt, ti) for ti in range(nt)] + ["identb"], writes=[ptt])
                P.I("dve", "tensor_scalar", hTb[:, k, 0:n], pt[:, 0:n],
                    modF[:, scale_j + k, s_:s_ + 1], modF[:, shift_j + k, s_:s_ + 1], ALU.mult, ALU.add,
                    reads=[ptt, "modF"], writes=[(hTt, k)])
            return hTb, [(hTt, k) for k in range(KD)]

        def gelu(src_ps, src_tok, dst_ap, dst_tok, n, extra_mul=None, extra_toks=()):
            a, at = R["tmpA"].next()
            b, bt = R["tmpB"].next()
            P.I("act", "activation", a[:, 0:n], src_ps, AF.Square, reads=[src_tok], writes=[at])
            P.I("dve", "tensor_scalar", a[:, 0:n], a[:, 0:n], 0.044715, 1.0, ALU.mult, ALU.add, reads=[at], writes=[at])
            P.I("dve", "tensor_tensor", a[:, 0:n], a[:, 0:n], src_ps, ALU.mult, reads=[at, src_tok], writes=[at])
            P.I("act", "activation", b[:, 0:n], a[:, 0:n], AF.Sigmoid, scale=1.5957691216057308, reads=[at], writes=[bt])
            if extra_mul is None:
                P.I("dve", "tensor_tensor", dst_ap, b[:, 0:n], src_ps, ALU.mult, reads=[bt, src_tok], writes=[dst_tok])
            else:
                P.I("dve", "tensor_tensor", b[:, 0:n], b[:, 0:n], src_ps, ALU.mult, reads=[bt, src_tok], writes=[bt])
                P.I("dve", "tensor_tensor", dst_ap, b[:, 0:n], extra_mul, ALU.mult, reads=[bt] + list(extra_toks), writes=[dst_tok])

        def rope_norm(zps, ztok, gcol, t0, n, dst_ap, dst_tok):
            sq, sqt = R["tbf"].next()
            zg, zgt = R["tbf2"].next()
            P.I("act", "activation", sq[:, 0:n], zps, AF.Square, reads=[ztok], writes=[sqt])
            P.I("act", "activation", zg[:, 0:n], zps, AF.Copy, scale=qkn[:, gcol:gcol + 1], reads=[ztok, "qkn"], writes=[zgt])
            pa, pat = ps_next()
            pb, pbt = ps_next()
            mm(pa[:, 0:n], [(blockones[:], sq[:, 0:n])], reads=[sqt, "blockones"], writes=[pat])
            mm(pb[:, 0:n], [(rotP[:], zg[:, 0:n])], reads=[zgt, "rotP"], writes=[pbt])
            cs, cst = R["rcs"].next()
            P.D("sp", [(cs[:, c_, 0:n], ropecs_d[c_, :, t0:t0 + n]) for c_ in range(2)], key=cst, writes=[cst])
            a, at = R["tmpA"].next()
            b, bt = R["tmpB"].next()
            c2, ct = R["tmpC"].next()
            P.I("act", "activation", a[:, 0:n], pa[:, 0:n], AF.Sqrt, bias=epsb[:], scale=1.0 / 64.0, reads=[pat, "epsb"], writes=[at])
            P.I("dve", "reciprocal", a[:, 0:n], a[:, 0:n], reads=[at], writes=[at])
            P.I("dve", "tensor_tensor", b[:, 0:n], zg[:, 0:n], cs[:, 0, 0:n], ALU.mult, reads=[zgt, cst], writes=[bt])
            P.I("dve", "tensor_tensor", c2[:, 0:n], pb[:, 0:n], cs[:, 1, 0:n], ALU.mult, reads=[pbt, cst], writes=[ct])
            P.I("dve", "tensor_tensor", b[:, 0:n], b[:, 0:n], c2[:, 0:n], ALU.add, reads=[bt, ct], writes=[bt])
            P.I("dve", "tensor_tensor", dst_ap, b[:, 0:n], a[:, 0:n], ALU.mult, reads=[bt, at], writes=[dst_tok])

        def wload(dst, dtok, src_ap, key):
            P.D("pool", [(dst, src_ap)], key=key, writes=[dtok])

        def post_norm_multi(tiles, gate_idx, g_idx, b_idx):
            st = []
            for (y_fn, dst_ap, dst_tok_d, src_ap, src_tok_d) in tiles:
                r, rt = R["r"].next()
                xt, xtt = R["xt"].next()
                xo, xot = R["xo"].next()
                P.D("sp", [(xt[:], src_ap)], key=xtt, reads=[src_tok_d] if src_tok_d else [], writes=[xtt])
                st.append((r, rt, xt, xtt, xo, xot))
            for (y_fn, dst_ap, dst_tok_d, src_ap, src_tok_d), (r, rt, xt, xtt, xo, xot) in zip(tiles, st):
                for cb in range(NF):
                    yap, ytok = y_fn(cb)
                    P.I("dve", "tensor_tensor", r[:, cb * 512:(cb + 1) * 512], yap, bcg[:, gate_idx, cb * 512:(cb + 1) * 512], ALU.mult,
                        reads=[ytok, "bcg"], writes=[(rt, cb)])
            for (r, rt, xt, xtt, xo, xot) in st:
                P.I("dve", "scalar_tensor_tensor", r[:], xt[:], cfg.ALPHA, r[:], ALU.mult, ALU.add,
                    reads=[xtt] + [(rt, cb) for cb in range(NF)], writes=[rt])
            ln_multi([(r[:], rt, xo[:], xot) for (r, rt, xt, xtt, xo, xot) in st])
            for (r, rt, xt, xtt, xo, xot) in st:
                P.I("dve", "tensor_tensor", xo[:], xo[:], lngb[:, g_idx, :], ALU.mult, reads=[xot, "lngb"], writes=[xot])
            for (r, rt, xt, xtt, xo, xot) in st:
                P.I("pool", "tensor_tensor", xo[:], xo[:], lngb[:, b_idx, :], ALU.add, reads=[xot, "lngb"], writes=[xot])
            for (y_fn, dst_ap, dst_tok_d, src_ap, src_tok_d), (r, rt, xt, xtt, xo, xot) in zip(tiles, st):
                P.D("sp", [(dst_ap, xo[:])], key=xot, reads=[xot], writes=[dst_tok_d])

        lat_blocks = [(b * 512, 512, 0) for b in range(S // 512)]
        ctx_block = (S, CTX, 1)

        SL = cfg.SL
        HALF_T = max(NT // 2, NCT)
        all_k = [("kT", h, b[0]) for h in range(2) for b in lat_blocks + [ctx_block]]

        def load_hT(blk):
            t0, n, s_ = blk
            hTb, hTt = R["hTb"].next()
            P.D("sp", [(hTb[:, :, 0:n], hT_d[:, :, t0:t0 + n])], key=("hTld", hTt),
                reads=[("hT_d", t0)], writes=[(hTt, k) for k in range(KD)])
            return hTb, [(hTt, k) for k in range(KD)]

        def modulation(l):
            scB, bmbc = T["scB"], T["bmbc"]
            for s_ in range(2):
                for k in range(KD):
                    P.I("dve", "tensor_scalar", scB[:, s_, k, :], zeros[:], sc32[:, k, s_:s_ + 1], None, ALU.add,
                        reads=["zeros", "sc32"], writes=[("scB", s_, k)])
            P.D("sp", [(bmF[:], b_modF[l])], key="bmF", writes=["bmF"])
            P.D("sp", [(lngb[:], ln_gb[l:l + 1].broadcast_to([128, 4, D]))], key="lngb", writes=["lngb"])
            P.D("sp", [(bmbc[:, 0, :], b_mod[l:l + 1, 2 * D:3 * D].broadcast_to([128, D])),
                       (bmbc[:, 1, :], b_mod[l:l + 1, 5 * D:6 * D].broadcast_to([128, D]))], key="bmbc", writes=["bmbc"])
            P.D("sp", [(qkn[:], qk_norm[l])], key="qkn", writes=["qkn"])
            wmv = w_mod[l].rearrange("(k p) n -> p k n", p=128)
            nslab = 6 * D // 512
            for sl in range(nslab):
                wm, wmt = R["wm"].next()
                wload(wm[:], wmt, wmv[:, :, sl * 512:(sl + 1) * 512], key=wmt)
                part = (sl * 512) // D
                if part in (2, 5):
                    gi = 0 if part == 2 else 1
                    col0 = sl * 512 - part * D
                    for s_ in range(2):
                        pb, pbt = ps_next()
                        mm(pb[:], [(scB[:, s_, k, :], wm[:, k, :]) for k in range(KD)],
                           reads=[wmt] + [("scB", s_, k) for k in range(KD)], writes=[pbt])
                        P.I("dve", "tensor_tensor", bcg[:, gi * 2 + s_, col0:col0 + 512], pb[:], bmbc[:, gi, col0:col0 + 512], ALU.add,
                            reads=[pbt, "bmbc"], writes=["bcg"])
                else:
                    pb, pbt = ps_next()
                    j0 = sl * 4
                    P.M("pe", [("matmul", (pb[:, jj * 2:jj * 2 + 2], wm[:, k, jj * 128:(jj + 1) * 128], scT[:, k, :]),
                                dict(start=(k == 0), stop=(k == KD - 1))) for jj in range(4) for k in range(KD)],
                        reads=[wmt, "scT"], writes=[pbt])
                    for s_ in range(2):
                        P.I("dve", "tensor_tensor", modF[:, j0:j0 + 4, s_],
                            pb[:, 0:8].rearrange("p (j s) -> p j s", s=2)[:, :, s_], bmF[:, j0:j0 + 4], ALU.add,
                            reads=[pbt, "bmF"], writes=["modF"])
            for base in (KD, 4 * KD):
                P.I("dve", "tensor_scalar", modF[:, base:base + KD, :], modF[:, base:base + KD, :], 1.0, None, ALU.add,
                    reads=["modF"], writes=["modF"])

        def phase_A_block(l, blk, wi, ctx_out):
            kT, V3, wA = T["kT"], T["V3"], T["wA"]
            wkd = wA[:, :, 0:256].rearrange("p k (h c) -> p k h c", h=2)
            wv = wA[:, :, 256:384]
            wft = wA[:, :, 512:1024]
            t0, n, s_ = blk
            nt = n // 128
            hTb, hTtoks = make_hT(blk, (lambda tile: x_src(l, tile)), 0, KD)
            P.D("sp", [(hT_d[:, :, t0:t0 + n], hTb[:, :, 0:n])], key=("hTst", hTtoks[0][0]), reads=hTtoks, writes=[("hT_d", t0)])
            for h in range(2):
                pz, pzt = ps_next()
                mm(pz[:, 0:n], [(wkd[:, k, h, :], hTb[:, k, 0:n]) for k in range(KD)], reads=hTtoks + ["wA"], writes=[pzt])
                rope_norm(pz[:, 0:n], pzt, 1, t0, n, kT[:, h, t0:t0 + n], ("kT", h, t0))
            for ti in range(nt):
                tile = t0 // 128 + ti
                pz, pzt = ps_next()
                mm(pz[:, 0:128], [(hTb[:, k, ti * 128:(ti + 1) * 128], wv[:, k, :]) for k in range(KD)], reads=hTtoks + ["wA"], writes=[pzt])
                pzv = pz[:, 0:128].rearrange("p (h c) -> p h c", h=2)
                P.I("act", "activation", V3[:, tile, :, 0:64], pzv, AF.Copy, reads=[pzt], writes=[("V3a", tile)])
                P.I("pool", "tensor_copy", V3[:, tile, :, 128:192], V3[:, tile, :, 0:64], reads=[("V3a", tile)], writes=[("V3b", tile)])
                if s_ == 0 or ctx_out:
                    pf, pft = ps_next()
                    mm(pf[:], [(hTb[:, k, ti * 128:(ti + 1) * 128], wft[:, k, :]) for k in range(KD)], reads=hTtoks + ["wA"], writes=[pft])
                    fb, fbt = R["tbf"].next()
                    P.I("act", "activation", fb[:], pf[:], AF.Copy, reads=[pft], writes=[fbt])
                    P.D("sp", [(ftok_d[tile], fb[:])], key=fbt, reads=[fbt], writes=[("ftok_d", tile)])

        def attn_prologue(blk):
            wq = T["wA"]
            t0, n, s_ = blk
            hTb, hTtoks = load_hT(blk)
            qs = []
            for c in range(4):
                pz, pzt = ps_next()
                mm(pz[:, 0:n], [(wq[:, k, c * 128:(c + 1) * 128], hTb[:, k, 0:n]) for k in range(KD)], reads=hTtoks + ["wA"], writes=[pzt])
                qT, qTt = R["qT"].next()
                rope_norm(pz[:, 0:n], pzt, 0, t0, n, qT[:, 0:n], qTt)
                qs.append((qT, qTt))
            return qs

        def attn_block(blk, qs, next_blk):
            kT, V3, rsum, bcs, rhl, sAB = T["kT"], T["V3"], T["rsum"], T["bcs"], T["rhl"], T["sAB"]
            t0, n, s_ = blk
            yb, ybt = R["yblk"].next()
            key_tiles = list(range(NT, TT)) + (list(range(NT)) if s_ == 0 else [])
            nk = len(key_tiles)
            poA, poAt, poB, poBt, pbc, pbct = PB[4], "po", PB[5], "pbc", PTF[0], ("pt", 0)
            pending = {}
            next_qs = [None]

            def emit_sT(c, ki):
                qT, qTt = qs[c]
                kv = c // 2
                kt = key_tiles[ki]
                pa, pat = ps_next()
                pb, pbt = ps_next()
                P.M("pe", [("matmul", (pa[:, 0:n], kT[0:64, kv, kt * 128:(kt + 1) * 128], qT[0:64, 0:n]), dict(start=True, stop=True)),
                           ("matmul", (pb[:, 0:n], kT[64:128, kv, kt * 128:(kt + 1) * 128], qT[64:128, 0:n]), dict(start=True, stop=True))],
                    reads=[qTt] + all_k, writes=[pat, pbt])
                pending[(c, ki)] = (pa, pat, pb, pbt)

            def fin_copy(c):
                P.I("act", "activation", sAB[:, 0, 0:n], poA[:, 0:n], AF.Copy, reads=[poAt], writes=[("sAB", 0)])
                P.I("dve", "tensor_copy", sAB[:, 1, 0:n], poB[:, 0:n], reads=[poBt], writes=[("sAB", 1)])
                P.I("dve", "reciprocal", rsum[64:128, 0:n], sAB[64:128, 0, 0:n], reads=[("sAB", 0)], writes=[("rsum", 0)])
                P.I("dve", "reciprocal", rsum[0:64, 0:n], sAB[0:64, 1, 0:n], reads=[("sAB", 1)], writes=[("rsum", 1)])
                P.I("dve", "tensor_copy", rhl[:, 0, 0:n], rsum[:, 0:n], reads=[("rsum", 0), ("rsum", 1)], writes=["rhi"])
                P.I("dve", "tensor_tensor", rhl[:, 1, 0:n], rsum[:, 0:n], rhl[:, 0, 0:n], ALU.subtract,
                    reads=[("rsum", 0), ("rsum", 1), "rhi"], writes=["rlo"])

            def fin_norm(c):
                P.M("pe", [("matmul", (pbc[:, 0:n], selb[:], rhl[:, 0, 0:n]), dict(start=True, stop=False)),
                           ("matmul", (pbc[:, 0:n], selb[:], rhl[:, 1, 0:n]), dict(start=False, stop=True))],
                    reads=["rhi", "rlo", "selb"], writes=[pbct])
                P.I("dve", "tensor_tensor", yb[0:64, c, 0:n], sAB[0:64, 0, 0:n], pbc[0:64, 0:n], ALU.mult,
                    reads=[("sAB", 0), pbct], writes=[(ybt, c, 0)])
                P.I("dve", "tensor_tensor", yb[64:128, c, 0:n], sAB[64:128, 1, 0:n], pbc[64:128, 0:n], ALU.mult,
                    reads=[("sAB", 1), pbct], writes=[(ybt, c, 1)])

            DEFER = 14
            emit_sT(0, 0)
            for c in range(4):
                kv = c // 2
                for ki in range(nk):
                    kt = key_tiles[ki]
                    pa, pat, pb, pbt = pending.pop((c, ki))
                    pTa, pTat = R["pT"].next()
                    pTb, pTbt = R["pT"].next()
                    P.I("act", "activation", pTa[:, 0:n], pa[:, 0:n], AF.Exp, scale=0.125, reads=[pat], writes=[pTat])
                    P.I("act", "activation", pTb[:, 0:n], pb[:, 0:n], AF.Exp, scale=0.125, reads=[pbt], writes=[pTbt])
                    if ki + 1 < nk:
                        emit_sT(c, ki + 1)
                    elif c + 1 < 4:
                        if c + 1 == 2 and next_blk is not None:
                            next_qs[0] = attn_prologue(next_blk)
                        emit_sT(c + 1, 0)
                    P.M("pe", [("matmul", (poA[:, 0:n], V3[:, kt, kv, 0:128], pTa[:, 0:n]), dict(start=(ki == 0), stop=(ki == nk - 1))),
                               ("matmul", (poB[:, 0:n], V3[:, kt, kv, 64:192], pTb[:, 0:n]), dict(start=(ki == 0), stop=(ki == nk - 1)))],
                        reads=[pTat, pTbt, ("V3a", kt), ("V3b", kt), "V3ones"], writes=[poAt, poBt])
                    if c > 0 and ki == min(DEFER, nk - 1):
                        fin_norm(c - 1)
                if c == 3:
                    fin_copy(c)
                    fin_norm(c)
                else:
                    fin_copy(c)
            P.D("sp", [(yb_d[0][:, :, t0:t0 + n], yb[:, :, 0:n])], key=("yst", ybt),
                reads=[(ybt, c, h_) for c in range(4) for h_ in range(2)], writes=[("y_d", 0, t0)])
            return next_qs[0]

        def sg_block(blk):
            wA, wB, sgbias = T["wA"], T["wB"], T["sgbias"]
            wsgv = wA[:, :, 0:512]
            wu_ = wA[:, :, 512:1024]
            wsT = wB
            t0, n, s_ = blk
            nt = n // 128
            hTb, hTtoks = load_hT(blk)
            mx, mxt = R["mx"].next()
            for ti in range(nt):
                pz, pzt = ps_next()
                mm(pz[:], [(hTb[:, k, ti * 128:(ti + 1) * 128], wsgv[:, k, :]) for k in range(KD)], reads=hTtoks + ["wA"], writes=[pzt])
                gv, gvt = R["gv"].next()
                gelu(pz[:], pzt, gv[:], gvt, 512)
                st, stt = R["stt"].next()
                mv, mvt = R["mv"].next()
                sd, sdt = R["sd"].next()
                rs, rst = R["rs"].next()
                P.M("dve", [("bn_stats", (st[:, g, :], gv[:, g * 128:(g + 1) * 128]), {}) for g in range(4)], reads=[gvt], writes=[stt])
                P.M("dve", [("bn_aggr", (mv[:, g, :], st[:, g, :]), {}) for g in range(4)], reads=[stt], writes=[mvt])
                P.I("act", "activation", sd[:], mv[:, :, 1], AF.Sqrt, bias=epsb[:], reads=[mvt, "epsb"], writes=[sdt])
                P.I("dve", "reciprocal", rs[:], sd[:], reads=[sdt], writes=[rst])
                vg, vgt = R["vg"].next()
                for g in range(4):
                    P.I("dve", "tensor_scalar", vg[:, g * 128:(g + 1) * 128], gv[:, g * 128:(g + 1) * 128], mv[:, g, 0:1], rs[:, g:g + 1],
                        ALU.subtract, ALU.mult, reads=[gvt, mvt, rst], writes=[(vgt, g)])
                pm, pmt = ps_next()
                P.M("pe", [("matmul", (pm[:, g * 128:(g + 1) * 128], vg[:, g * 128:(g + 1) * 128], wsT[:, g, :]), dict(start=True, stop=True))
                           for g in range(4)], reads=[(vgt, g) for g in range(4)] + ["wB"], writes=[pmt])
                P.I("dve", "tensor_tensor", mx[:, :, ti * 128:(ti + 1) * 128], pm[:].rearrange("p (g c) -> p g c", g=4),
                    sgbias[:].rearrange("p (g c) -> p g c", g=4), ALU.add, reads=[pmt, "sgbias"], writes=[(mxt, ti)])
            yb, ybt = R["yblk"].next()
            for g in range(4):
                pu, put = ps_next()
                mm(pu[:, 0:n], [(wu_[:, k, g * 128:(g + 1) * 128], hTb[:, k, 0:n]) for k in range(KD)], reads=hTtoks + ["wA"], writes=[put])
                gelu(pu[:, 0:n], put, yb[:, g, 0:n], (ybt, g), n, extra_mul=mx[:, g, 0:n], extra_toks=[(mxt, ti) for ti in range(nt)])
            P.D("sp", [(yb_d[1][:, :, t0:t0 + n], yb[:, :, 0:n])], key=("yst", ybt),
                reads=[(ybt, g) for g in range(4)], writes=[("y_d", 1, t0)])

        def fourier(tile0, ntile, table, nkb, kw, tokbase):
            ftok_sb, Gsb = T["ftok_sb"], T["Gsb"]
            Ltot = ntile * 128
            scale = float(1.0 / np.sqrt(Ltot * 128.0))
            P.D("sp", [(ftok_sb[:, i, :], ftok_d[tile0 + i]) for i in range(ntile)],
                key="ftok_sb", reads=[("ftok_d", tile0 + i) for i in range(ntile)], writes=["ftok_sb"])
            for kb in range(nkb):
                for lc in range(ntile):
                    dt_, dtt = R["dft"].next()
                    P.D("sp", [(dt_[:, :, 0:kw], table[lc, kb])], key=dtt, writes=[dtt])
                    P.M("pe", [("matmul", (PB[g][:, cs_ * 256:cs_ * 256 + kw], ftok_sb[:, lc, g * 128:(g + 1) * 128], dt_[:, cs_, 0:kw]),
                                dict(start=(lc == 0 and cs_ == 0), stop=(lc == ntile - 1), skip_group_check=True)) for g in range(4) for cs_ in range(2)],
                        reads=[dtt, "ftok_sb"], writes=[("ps", g) for g in range(4)])
                for g in range(4):
                    src = PB[g][:].rearrange("p (c k) -> p c k", c=2)[:, :, 0:kw]
                    if g % 2 == 0:
                        P.I("act", "activation", Gsb[:, g, :, 0:kw], src, AF.Copy, reads=[("ps", g)], writes=[("Gsb", g)])
                    else:
                        P.I("dve", "tensor_copy", Gsb[:, g, :, 0:kw], src, reads=[("ps", g)], writes=[("Gsb", g)])
                yf, yft = R["yfr"].next()
                for g in range(4):
                    po_ = PB[4] if g % 2 == 0 else PB[5]
                    pot = "po" if g % 2 == 0 else "pbc"
                    mm(po_[:, 0:kw], [(c128[:, 0, :], Gsb[:, g, 0, 0:kw]), (c128[:, 1, :], Gsb[:, g, 1, 0:kw])],
                       reads=[("Gsb", g), "c128"], writes=[pot])
                    P.I("act", "activation", yf[:, g, 0:kw], po_[:, 0:kw], AF.Copy, scale=scale, reads=[pot], writes=[(yft, g)])
                tb = tokbase + kb * kw
                P.D("sp", [(yb_d[2][:, :, tb:tb + kw], yf[:, :, 0:kw])], key=("yfst", yft),
                    reads=[(yft, g) for g in range(4)], writes=[("y_d", 2, tb)])

        def merge_loads(blk):
            t0, n, s_ = blk
            hTb, hTtoks = load_hT(blk)
            ys = []
            for n_ in range(3):
                yb, ybt = R["ybs%d" % n_].next()
                if n_ == 2:
                    rd = [("y_d", 2, t0 + i * 256) for i in range(n // 256)] if s_ == 0 else [("y_d", 2, t0)]
                else:
                    rd = [("y_d", n_, t0)]
                P.D("sp", [(yb[:, :, 0:n], yb_d[n_][:, :, t0:t0 + n])], key=("ybld", ybt), reads=rd, writes=[ybt])
                ys.append((yb, ybt))
            return hTb, hTtoks, ys

        def merge_block(l, blk, loaded, next_blk):
            wA, wB, wC = T["wA"], T["wB"], T["wC"]
            wgate = wA
            t0, n, s_ = blk
            nt = n // 128
            hTb, hTtoks, ys = loaded
            mT, mTt = R["mT"].next()
            for dc in range(KD):
                acc, acct = R["acc"].next()
                for n_ in range(3):
                    yb, ybt = ys[n_]
                    pp, ppt = ps_next()
                    mm(pp[:, 0:n], [(wB[:, n_, c, dc * 128:(dc + 1) * 128], yb[:, c, 0:n]) for c in range(4)], reads=[ybt, "wB"], writes=[ppt])
                    pg, pgt = ps_next()
                    mm(pg[:, 0:n], [(wgate[:, k, n_ * D + dc * 128:n_ * D + (dc + 1) * 128], hTb[:, k, 0:n]) for k in range(KD)],
                       reads=hTtoks + ["wA"], writes=[pgt])
                    sg, sgt = R["tmpA"].next()
                    P.I("act", "activation", sg[:, 0:n], pg[:, 0:n], AF.Sigmoid, reads=[pgt], writes=[sgt])
                    if n_ == 0:
                        P.I("dve", "tensor_tensor", acc[:, 0:n], sg[:, 0:n], pp[:, 0:n], ALU.mult, reads=[sgt, ppt], writes=[acct])
                    else:
                        P.I("dve", "tensor_tensor", sg[:, 0:n], sg[:, 0:n], pp[:, 0:n], ALU.mult, reads=[sgt, ppt], writes=[sgt])
                        if n_ == 1:
                            P.I("pool", "tensor_tensor", acc[:, 0:n], acc[:, 0:n], sg[:, 0:n], ALU.add, reads=[sgt, acct], writes=[acct])
                        else:
                            P.I("dve", "tensor_tensor", mT[:, dc, 0:n], acc[:, 0:n], sg[:, 0:n], ALU.add, reads=[sgt, acct], writes=[(mTt, dc)])
            nxt = merge_loads(next_blk) if next_blk is not None else None
            for tg in range(0, nt, 2):
                grp = []
                for ti in range(tg, min(tg + 2, nt)):
                    tile = t0 // 128 + ti
                    pys = []
                    for cb in range(NF):
                        py, pyt = ps_next()
                        mm(py[:], [(mT[:, dc, ti * 128:(ti + 1) * 128], wC[:, dc, cb * 512:(cb + 1) * 512]) for dc in range(KD)],
                           reads=[(mTt, dc) for dc in range(KD)] + ["wC"], writes=[pyt])
                        pys.append((py[:], pyt))
                    src, srctok = x_src(l, tile)
                    grp.append(((lambda cb, pys=pys: pys[cb]), x1_d[tile * 128:(tile + 1) * 128, :], ("x1_d", tile), src, srctok))
                post_norm_multi(grp, 0 + s_, 0, 1)
            return nxt

        def router_tile(hTb, hTtoks, ti, lt):
            wr, gates = T["wr"], T["gates"]
            pr, prt = ps_next()
            mm(pr[:, 0:NE], [(hTb[:, k, ti * 128:(ti + 1) * 128], wr[:, k, 0:NE]) for k in range(KD)], reads=hTtoks + ["wr"], writes=[prt])
            lg, lgt = R["tmpA"].next()
            m8, m8t = R["tmpB"].next()
            P.I("dve", "tensor_copy", lg[:, 0:8], pr[:, 0:8], reads=[prt], writes=[lgt])
            P.I("dve", "max", m8[:, 0:8], lg[:, 0:8], reads=[lgt], writes=[m8t])
            P.I("dve", "tensor_tensor", m8[:, 8:9], m8[:, 0:1], m8[:, 1:2], ALU.subtract, reads=[m8t], writes=[(m8t, "d")])
            P.I("act", "activation", m8[:, 9:10], m8[:, 8:9], AF.Sigmoid, reads=[(m8t, "d")], writes=[(m8t, "w1")])
            P.I("act", "activation", m8[:, 10:11], m8[:, 8:9], AF.Sigmoid, scale=-1.0, reads=[(m8t, "d")], writes=[(m8t, "w2")])
            P.I("dve", "tensor_scalar", lg[:, 8:16], lg[:, 0:8], m8[:, 0:1], m8[:, 9:10], ALU.is_equal, ALU.mult,
                reads=[lgt, m8t, (m8t, "w1")], writes=[(lgt, "g1")])
            P.I("dve", "tensor_scalar", lg[:, 16:24], lg[:, 0:8], m8[:, 1:2], m8[:, 10:11], ALU.is_equal, ALU.mult,
                reads=[lgt, m8t, (m8t, "w2")], writes=[(lgt, "g2")])
            P.I("dve", "tensor_tensor", gates[:, lt, :], lg[:, 8:16], lg[:, 16:24], ALU.add,
                reads=[(lgt, "g1"), (lgt, "g2")], writes=[("gates", lt)])

        def ffn(l, ctx_out, last):
            moe = (l % 2 == 1)
            li = l // 2
            if moe:
                nexp, dff = NE, cfg.DFFE
            else:
                nexp, dff = 1, cfg.DFF
            nch = dff // 128
            slabs = [(c0, min(SL, nch - c0)) for c0 in range(0, nch, SL)]
            half_tiles = NT // 2
            bph = half_tiles // 4
            tok_sets = [[(b_ * 512, 512, 0) for b_ in range(h_ * bph, (h_ + 1) * bph)] for h_ in range(2)]
            if ctx_out:
                tok_sets.append([ctx_block])
            fixed = [("yacc", [128, HALF_T, D], F32), ("hT2", [128, KD, HALF_T * 128], BF16),
                     ("gates", [128, HALF_T, 8], F32), ("wr", [128, KD, 8], BF16)]
            for tset in tok_sets:
                base_tok = tset[0][0]
                phase(HT_RINGS + ("tmpA", "tmpB"), fixed)
                keep = ar["off"] if False else None
                yacc, hT2, gates, wr = T["yacc"], T["hT2"], T["gates"], T["wr"]
                fixed_bytes = sum(((int(np.prod(shp[1:])) * (4 if dt == F32 else 2) + 63) // 64 * 64) for _, shp, dt in fixed)
                P.barrier()
                ar["off"] = 0
                for nm, shp, dt in fixed:
                    T[nm] = a_alloc(shp, dt)
                yacc, hT2, gates, wr = T["yacc"], T["hT2"], T["gates"], T["wr"]
                for k_ in list(R.keys()):
                    del R[k_]
                for nm in HT_RINGS + ("tmpA", "tmpB"):
                    make_ring(nm)
                if moe:
                    wload(wr[:, :, 0:NE], "wr", router[li].rearrange("(k p) e -> p k e", p=128), key="wr")
                for blk in tset:
                    t0, n, s_ = blk
                    nt = n // 128
                    hTb, hTtoks = make_hT(blk, (lambda tile: (x1_d[tile * 128:(tile + 1) * 128, :], ("x1_d", tile))), 3 * KD, 4 * KD)
                    off = t0 - base_tok
                    P.I("pool", "tensor_copy", hT2[:, :, off:off + n], hTb[:, :, 0:n], reads=hTtoks, writes=[("hT2", t0)])
                    if moe:
                        for ti in range(nt):
                            router_tile(hTb, hTtoks, ti, off // 128 + ti)
                P.barrier()
                ar["off"] = fixed_bytes
                for k_ in list(R.keys()):
                    del R[k_]
                for nm in ("wgu", "wd", "actT", "tmpC"):
                    make_ring(nm)
                first = True
                for e_ in range(nexp):
                    if moe:
                        wg_, wu2, wd2 = exp_wg[li, e_], exp_wu[li, e_], exp_wd[li, e_]
                    else:
                        wg_, wu2, wd2 = ffn_wg[li], ffn_wu[li], ffn_wd[li]
                    wgv = wg_.rearrange("(k p) f -> p k f", p=128)
                    wuv = wu2.rearrange("(k p) f -> p k f", p=128)
                    wdv = wd2.rearrange("(c p) d -> p c d", p=128)
                    for (c0, ncs) in slabs:
                        wgu, wgut = R["wgu"].next()
                        wd_, wdt = R["wd"].next()
                        P.D("pool", [(wgu[:, :, 0, 0:ncs * 128], wgv[:, :, c0 * 128:(c0 + ncs) * 128]),
                                     (wgu[:, :, 1, 0:ncs * 128], wuv[:, :, c0 * 128:(c0 + ncs) * 128])], key=wgut, writes=[wgut])
                        P.D("pool", [(wd_[:, 0:ncs, :], wdv[:, c0:c0 + ncs, :])], key=wdt, writes=[wdt])
                        for blk in tset:
                            t0, n, s_ = blk
                            nt = n // 128
                            off = t0 - base_tok
                            aT, aTt = R["actT"].next()
                            for ch in range(ncs):
                                pg, pgt = ps_next()
                                pu, put = ps_next()
                                mm(pg[:, 0:n], [(wgu[:, k, 0, ch * 128:(ch + 1) * 128], hT2[:, k, off:off + n]) for k in range(KD)],
                                   reads=[wgut, ("hT2", t0)], writes=[pgt])
                                mm(pu[:, 0:n], [(wgu[:, k, 1, ch * 128:(ch + 1) * 128], hT2[:, k, off:off + n]) for k in range(KD)],
                                   reads=[wgut, ("hT2", t0)], writes=[put])
                                sg, sgt = R["tmpC"].next()
                                P.I("act", "activation", sg[:, 0:n], pg[:, 0:n], AF.Silu, reads=[pgt], writes=[sgt])
                                P.I("dve", "tensor_tensor", aT[:, ch, 0:n], sg[:, 0:n], pu[:, 0:n], ALU.mult, reads=[sgt, put], writes=[(aTt, ch)])
                            for ti in range(nt):
                                lt = off // 128 + ti
                                for cb in range(NF):
                                    py, pyt = ps_next()
                                    mm(py[:], [(aT[:, ch, ti * 128:(ti + 1) * 128], wd_[:, ch, cb * 512:(cb + 1) * 512]) for ch in range(ncs)],
                                       reads=[(aTt, ch) for ch in range(ncs)] + [wdt], writes=[pyt])
                                    ya = yacc[:, lt, cb * 512:(cb + 1) * 512]
                                    yat = ("yacc", lt, cb)
                                    if moe:
                                        gsc = gates[:, lt, e_:e_ + 1]
                                        if first:
                                            P.I("dve", "tensor_scalar", ya, py[:], gsc, None, ALU.mult, reads=[pyt, ("gates", lt)], writes=[yat])
                                        else:
                                            P.I("dve", "scalar_tensor_tensor", ya, py[:], gsc, ya, ALU.mult, ALU.add,
                                                reads=[pyt, ("gates", lt), yat], writes=[yat])
                                    else:
                                        if first:
                                            P.I("act", "activation", ya, py[:], AF.Copy, reads=[pyt], writes=[yat])
                                        else:
                                            P.I("dve", "tensor_tensor", ya, ya, py[:], ALU.add, reads=[pyt, yat], writes=[yat])
                        first = False
                P.barrier()
                ar["off"] = fixed_bytes
                for k_ in list(R.keys()):
                    del R[k_]
                for nm in (("xt", 4), ("r", 4), ("xo", 4)) + LN_RINGS:
                    make_ring(nm)
                for blk in tset:
                    t0, n, s_ = blk
                    if last and s_ == 1:
                        continue
                    grp = []
                    for ti in range(n // 128):
                        tile = t0 // 128 + ti
                        lt = (t0 - base_tok) // 128 + ti
                        if last:
                            dst, dtok = out_d[tile * 128:(tile + 1) * 128, :], ("out", tile)
                        else:
                            dst, dtok = x2_d[l][tile * 128:(tile + 1) * 128, :], ("x2_d", l, tile)
                        grp.append(((lambda cb, lt=lt: (yacc[:, lt, cb * 512:(cb + 1) * 512], ("yacc", lt, cb))),
                                    dst, dtok, x1_d[tile * 128:(tile + 1) * 128, :], ("x1_d", tile)))
                    post_norm_multi(grp, 2 + s_, 2, 3)

        KV_T = [("kT", [128, 2, TOT], BF16), ("V3", [128, TT, 2, 192], BF16)]
        for l in range(cfg.EMIT_LAYERS):
            last = (l == L - 1)
            ctx_out = not last
            phase(("wm",), [("scB", [128, 2, KD, 128], BF16), ("bmbc", [128, 2, D], F32)])
            modulation(l)
            if cfg.STOP == "mod":
                break
            wi = w_in[l].rearrange("(k p) n -> p k n", p=128)
            phase(HT_RINGS + ("tmpA", "tmpB", "tmpC", "tbf", "tbf2", "rcs"), KV_T + [("wA", [128, KD, 1024], BF16)])
            wA = T["wA"]
            P.I("pool", "memset", T["V3"][:, :, :, 64:128], 1.0, writes=["V3ones"])
            for h in range(2):
                for dup in range(2):
                    c0 = h * 128 + dup * 64
                    wload(wA[:, :, c0:c0 + 64], "wA", wi[:, :, cfg.OFF_K + h * 64:cfg.OFF_K + h * 64 + 64], key=("wA", h, dup))
            wload(wA[:, :, 256:384], "wA", wi[:, :, cfg.OFF_V:cfg.OFF_V + 128], key=("wA", 9))
            wload(wA[:, :, 512:1024], "wA", wi[:, :, cfg.OFF_FT:cfg.OFF_FT + 512], key=("wA", 10))
            for blk in lat_blocks + [ctx_block]:
                phase_A_block(l, blk, wi, ctx_out)
            q_blocks = lat_blocks + ([ctx_block] if ctx_out else [])
            if cfg.STOP == "A":
                break
            phase(("hTb", "yblk", "qT", "pT", "tmpA", "tmpB", "tmpC", "tbf", "tbf2", "rcs"),
                  KV_T + [("wA", [128, KD, 512], BF16), ("rsum", [128, 512], F32), ("bcs", [128, 512], F32), ("rhl", [128, 2, 512], BF16), ("sAB", [128, 2, 512], F32)])
            wload(T["wA"][:], "wA", wi[:, :, cfg.OFF_Q:cfg.OFF_Q + 512], key=("wA", 11))
            qs_cur = attn_prologue(q_blocks[0])
            for bi, blk in enumerate(q_blocks):
                nb_ = q_blocks[bi + 1] if bi + 1 < len(q_blocks) else None
                qs_cur = attn_block(blk, qs_cur, nb_)
            if cfg.STOP == "B1":
                break
            phase(("hTb", "yblk", "mx", "gv", "vg", "tmpA", "tmpB") + LN_RINGS,
                  [("wA", [128, KD, 1024], BF16), ("wB", [128, 4, 128], BF16), ("sgbias", [128, 512], F32)])
            wload(T["wA"][:, :, 0:512], "wA", wi[:, :, cfg.OFF_SGV:cfg.OFF_SGV + 512], key=("wA", 12))
            wload(T["wA"][:, :, 512:1024], "wA", wi[:, :, cfg.OFF_U:cfg.OFF_U + 512], key=("wA", 13))
            wload(T["wB"][:], "wB", sg_wT[l], key=("wB", 0))
            P.D("sp", [(T["sgbias"][:], sg_b[l:l + 1, :].broadcast_to([128, 512]))], key="sgbias", writes=["sgbias"])
            for blk in q_blocks:
                sg_block(blk)
            if cfg.STOP == "B2":
                break
            phase(("dft", "yfr"), [("ftok_sb", [128, NT, 512], BF16), ("Gsb", [128, 4, 2, 256], BF16)])
            fourier(0, NT, dftL_d, cfg.NKB, 256, 0)
            if ctx_out:
                fourier(NT, NCT, dftC_d, 1, CTX, S)
            if cfg.STOP == "B3":
                break
            phase(("hTb", "ybs0", "ybs1", "ybs2", "mT", "acc", "tmpA", "xt", "r", "xo") + LN_RINGS,
                  [("wA", [128, KD, 3 * D], BF16), ("wB", [128, 3, 4, D], BF16), ("wC", [128, KD, D], BF16)])
            wload(T["wA"][:], "wA", wi[:, :, cfg.OFF_GATE:cfg.OFF_GATE + 3 * D], key=("wA", 14))
            wload(T["wB"][:], "wB", w_branch[l].rearrange("n (c p) d -> p n c d", p=128), key=("wB", 1))
            wload(T["wC"][:], "wC", w_out[l].rearrange("(k p) d -> p k d", p=128), key=("wC", 0))
            ld_cur = merge_loads(q_blocks[0])
            for bi, blk in enumerate(q_blocks):
                nb_ = q_blocks[bi + 1] if bi + 1 < len(q_blocks) else None
                ld_cur = merge_block(l, blk, ld_cur, nb_)
            if cfg.STOP == "B4":
                break
            ffn(l, ctx_out, last)

        if cfg.MAXOPS is not None:
            print("total ops", len(P.ops))
            P.ops = P.ops[:cfg.MAXOPS]
            print("last op:", P.ops[-1].eng, P.ops[-1].reads, P.ops[-1].writes)
        stats = P.emit(final_waits=[("xo", i) for i in range(4)] if (cfg.STOP is None and cfg.MAXOPS is None) else [])
    return nc, stats


def make_in_maps(cfg, inputs, nb):
    D, KD = cfg.D, cfg.KD
    L = cfg.DEPTH
    consts = make_consts(cfg)
    f = lambda a: np.ascontiguousarray(np.asarray(a, dtype=np.float32))
    shared = dict(consts)
    shared["w_mod"] = f(inputs["w_mod"])
    bm = f(inputs["b_mod"])
    shared["b_mod"] = bm
    shared["b_modF"] = np.ascontiguousarray(bm.reshape(L, 6 * KD, 128).transpose(0, 2, 1))
    shared["w_in"] = f(inputs["w_in"])
    qn = np.tile(f(inputs["q_norm"]), (1, 2))
    kn = np.tile(f(inputs["k_norm"]), (1, 2))
    shared["qk_norm"] = np.ascontiguousarray(np.stack([qn, kn], axis=-1))
    shared["sg_wT"] = np.ascontiguousarray(f(inputs["sg_w"]).transpose(0, 3, 1, 2))
    shared["sg_b"] = np.ascontiguousarray(f(inputs["sg_b"]).reshape(L, 512))
    shared["w_branch"] = f(inputs["w_branch"])
    shared["w_out"] = f(inputs["w_out"])
    shared["ln_gb"] = np.ascontiguousarray(np.stack([f(inputs["ln1_g"]), f(inputs["ln1_b"]), f(inputs["ln2_g"]), f(inputs["ln2_b"])], axis=1))
    shared["ffn_w_gate"] = f(inputs["ffn_w_gate"])
    shared["ffn_w_up"] = f(inputs["ffn_w_up"])
    shared["ffn_w_down"] = f(inputs["ffn_w_down"])
    if L // 2:
        shared["router"] = f(inputs["router"])
        shared["exp_w_gate"] = f(inputs["exp_w_gate"])
        shared["exp_w_up"] = f(inputs["exp_w_up"])
        shared["exp_w_down"] = f(inputs["exp_w_down"])
    x = f(inputs["x"])
    ctx = f(inputs["ctx"])
    c = f(inputs["c"])
    cc = f(inputs["c_ctx"])
    maps = []
    for b in range(nb):
        m = dict(shared)
        m["x"] = x[b]
        m["ctx"] = ctx[b]
        cv = np.stack([c[b].reshape(KD, 128).T, cc.reshape(KD, 128).T], axis=-1)
        m["cvecs"] = np.ascontiguousarray(cv)
        maps.append(m)
    return maps


_CACHE = {}


def kernel(**inputs):
    cfg = FULL
    nb = inputs["x"].shape[0]
    if "nc" not in _CACHE:
        _CACHE["nc"] = build_program(cfg)[0]
    nc = _CACHE["nc"]
    maps = make_in_maps(cfg, inputs, nb)
    res = run_bass_kernel_spmd(nc, maps, core_ids=list(range(nb)))
    out = np.stack([np.asarray(r["out"], dtype=np.float32) for r in res.results], axis=0)
    return out
```

```python
import contextlib
import numpy as np
import ml_dtypes
import concourse.bass as bass
import concourse.mybir as mybir
from concourse.bass_utils import run_bass_kernel_spmd

F32 = mybir.dt.float32
BF16 = mybir.dt.bfloat16
ALU = mybir.AluOpType
AF = mybir.ActivationFunctionType

ENGS = ("pe", "act", "dve", "pool", "sp")


class Op:
    __slots__ = ("eng", "fn", "reads", "writes", "dma", "ndma", "key", "deps",
                 "needs_inc", "sem", "val", "waits", "extra")

    def __init__(self, eng, fn, reads, writes, dma=False, ndma=1, key=None):
        self.eng = eng
        self.fn = fn
        self.reads = tuple(reads)
        self.writes = tuple(writes)
        self.dma = dma
        self.ndma = ndma
        self.key = key
        self.deps = ()
        self.needs_inc = False
        self.sem = None
        self.val = 0
        self.waits = ()
        self.extra = ()


def is_psum(t):
    return t in ("po", "pbc") or (isinstance(t, tuple) and len(t) == 2 and t[0] in ("ps", "pt"))


class Prog:
    def __init__(self, nc):
        self.nc = nc
        self.ops = []
        self.last_eng = {}
        self.last_key = {}
        self.bar = ()
        self.bar_pending = set()

    def barrier(self):
        self.bar = tuple(self.last_eng.values()) + tuple(self.last_key.values())
        self.bar_pending = set(ENGS)

    def _track(self, o):
        i = len(self.ops)
        if o.eng in self.bar_pending:
            o.extra = self.bar
            self.bar_pending.discard(o.eng)
        self.ops.append(o)
        if o.dma:
            self.last_key[o.key] = i
        else:
            self.last_eng[o.eng] = i

    def I(self, eng, method, *args, reads=(), writes=(), **kw):
        return self.M(eng, [(method, args, kw)], reads, writes)

    def M(self, eng, insts, reads=(), writes=()):
        insts = list(insts)

        def fn(e):
            r = None
            for (m, a, kw) in insts:
                r = getattr(e, m)(*a, **kw)
            return r
        o = Op(eng, fn, reads, writes)
        self._track(o)
        return o

    def D(self, eng, pairs, key, reads=(), writes=()):
        pairs = list(pairs)

        def fn(e):
            return [e.dma_start(out=o_, in_=i_) for (o_, i_) in pairs]
        o = Op(eng, fn, reads, tuple(writes) + (("dmakey", key),), dma=True, ndma=len(pairs), key=key)
        self._track(o)
        return o

    def analyze(self):
        last_w = {}
        readers = {}
        ops = self.ops
        for i, o in enumerate(ops):
            deps = set()
            for t in o.reads:
                w = last_w.get(t)
                if w is not None:
                    deps.add(w)
                if is_psum(t):
                    for r in readers.get(t, ()):
                        if ops[r].eng != o.eng:
                            deps.add(r)
            for t in o.writes:
                w = last_w.get(t)
                if w is not None:
                    deps.add(w)
                for r in readers.get(t, ()):
                    deps.add(r)
            deps.update(o.extra)
            deps.discard(i)
            keep = []
            for j in deps:
                d = ops[j]
                if d.dma:
                    keep.append(j)
                    continue
                if d.eng == o.eng and not o.dma:
                    if o.eng == "pe":
                        continue
                keep.append(j)
            o.deps = keep
            for j in keep:
                ops[j].needs_inc = True
            for t in o.reads:
                readers.setdefault(t, []).append(i)
            for t in o.writes:
                last_w[t] = i
                readers[t] = []

    def emit(self, final_waits=()):
        nc = self.nc
        self.analyze()
        ops = self.ops
        keys = []
        seen = set()
        for o in ops:
            if o.dma and o.key not in seen:
                seen.add(o.key)
                keys.append(o.key)
        with contextlib.ExitStack() as es:
            esem = {e: es.enter_context(nc.semaphore("s_" + e)) for e in ENGS}
            ksem = {k: es.enter_context(nc.semaphore("k%d" % n)) for n, k in enumerate(keys)}
            cnt = {e: 0 for e in ENGS}
            kcnt = {k: 0 for k in keys}
            for o in ops:
                if o.dma:
                    kcnt[o.key] += 16 * o.ndma
                    o.sem = ksem[o.key]
                    o.val = kcnt[o.key]
                elif o.needs_inc:
                    cnt[o.eng] += 1
                    o.sem = esem[o.eng]
                    o.val = cnt[o.eng]
            known = {e: {} for e in ENGS}
            for o in ops:
                w = {}
                for j in o.deps:
                    d = ops[j]
                    sid = id(d.sem)
                    if sid not in w or w[sid][1] < d.val:
                        w[sid] = (d.sem, d.val)
                kn = known[o.eng]
                ws = []
                for sid, (s, v) in w.items():
                    if kn.get(sid, 0) >= v:
                        continue
                    kn[sid] = v
                    ws.append((s, v))
                o.waits = ws
            per_eng = {e: [o for o in ops if o.eng == e] for e in ENGS}
            last_dma = {}
            for o in ops:
                if o.dma:
                    last_dma[o.key] = o
            block = es.enter_context(nc.Block())

            def run(eng_name, eng):
                for o in per_eng[eng_name]:
                    for s, v in o.waits:
                        eng.wait_ge(s, v)
                    r = o.fn(eng)
                    if o.dma:
                        rs = r if isinstance(r, (list, tuple)) else [r]
                        assert len(rs) == o.ndma, (len(rs), o.ndma)
                        for ins in rs:
                            ins.then_inc(o.sem, 16)
                    elif o.needs_inc:
                        r.then_inc(o.sem, 1)
                if eng_name == "sp":
                    for k in (final_waits if final_waits else list(last_dma.keys())):
                        if k in last_dma:
                            o = last_dma[k]
                            eng.wait_ge(o.sem, o.val)

            @block.tensor
            def _(e):
                run("pe", e)

            @block.scalar
            def _(e):
                run("act", e)

            @block.vector
            def _(e):
                run("dve", e)

            @block.gpsimd
            def _(e):
                run("pool", e)

            @block.sync
            def _(e):
                run("sp", e)
        return {e: len(per_eng[e]) for e in ENGS}, len(keys)


class Cfg:
    def __init__(self, D=1024, S=4096, CTX=256, DFF=2816, NE=8, DFFE=3584, DEPTH=2, GRID_W=64, SL=4):
        self.D, self.S, self.CTX, self.DFF, self.NE, self.DFFE = D, S, CTX, DFF, NE, DFFE
        self.DEPTH, self.GRID_W, self.SL = DEPTH, GRID_W, SL
        self.KD = D // 128
        self.NT = S // 128
        self.NCT = CTX // 128
        self.TOT = S + CTX
        self.TT = self.NT + self.NCT
        self.OFF_Q, self.OFF_K, self.OFF_V, self.OFF_U = 0, 512, 640, 768
        self.OFF_SGV, self.OFF_FT, self.OFF_GATE = 1280, 1792, 2304
        self.INW = 2304 + 3 * D
        self.ALPHA = float((2 * DEPTH) ** 0.25)
        self.BW = 512
        self.NKB = S // 256
        self.ARENA_KB = 166
        self.EMIT_LAYERS = DEPTH
        self.STOP = None
        self.MAXOPS = None


FULL = Cfg()


def make_consts(cfg):
    bf = ml_dtypes.bfloat16
    S, CTX, TOT, GW = cfg.S, cfg.CTX, cfg.TOT, cfg.GRID_W
    c = {}
    c["identb"] = np.eye(128, dtype=np.float32).astype(bf)
    P = np.zeros((128, 128), np.float32)
    for m in range(128):
        if (m % 32) < 16:
            P[m + 16, m] = -1.0
        else:
            P[m - 16, m] = 1.0
    c["rotP"] = P.astype(bf)
    B = np.zeros((128, 128), np.float32)
    B[:64, :64] = 1.0
    B[64:, 64:] = 1.0
    c["blockones"] = B.astype(bf)
    sel = np.zeros((128, 128), np.float32)
    sel[64, 0:64] = 1.0
    sel[0, 64:128] = 1.0
    c["onesb"] = sel.astype(bf)
    t = np.arange(S)
    row = (t // GW).astype(np.float64)
    col = (t % GW).astype(np.float64)
    i = np.arange(128) % 64
    j = i % 16
    inv = 10000.0 ** (-(j.astype(np.float64)) / 16.0)
    pos = np.where((i < 32)[:, None], row[None, :], col[None, :])
    ang = pos * inv[:, None]
    cs = np.zeros((2, 128, TOT), np.float32)
    cs[0, :, :S] = np.cos(ang)
    cs[1, :, :S] = np.sin(ang)
    cs[0, :, S:] = 1.0
    c["ropecs"] = cs

    def dft_table(L, nkb, kw):
        l = np.arange(L, dtype=np.int64)
        k = np.arange(L, dtype=np.int64)
        ph = (np.outer(l, k) % L).astype(np.float64) * (2.0 * np.pi / L)
        C = np.cos(ph).astype(np.float32)
        Sn = np.sin(ph).astype(np.float32)
        tab = np.stack([C, Sn], axis=1)
        tab = tab.reshape(L // 128, 128, 2, nkb, kw).transpose(0, 3, 1, 2, 4)
        return np.ascontiguousarray(tab).astype(bf)

    c["dftL"] = dft_table(S, cfg.NKB, 256)
    c["dftC"] = dft_table(CTX, 1, CTX)
    d = np.arange(128, dtype=np.int64)
    ph = (np.outer(d, d) % 128).astype(np.float64) * (2.0 * np.pi / 128)
    c["c128"] = np.stack([np.cos(ph), -np.sin(ph)], axis=1).astype(np.float32).astype(bf)
    return c


def build_program(cfg, debug_outs=()):
    nc = bass.Bass("TRN2", target_bir_lowering=False)
    D, S, CTX, TOT, KD, NT, NCT, TT = cfg.D, cfg.S, cfg.CTX, cfg.TOT, cfg.KD, cfg.NT, cfg.NCT, cfg.TT
    L, NE, INW = cfg.DEPTH, cfg.NE, cfg.INW
    ND = (L + 1) // 2
    NM = L // 2

    def din(name, shape, dt=F32):
        return nc.dram_tensor(name, list(shape), dt, kind="ExternalInput").ap()

    def dscr(name, shape, dt):
        kind = "ExternalOutput" if name in debug_outs else "Internal"
        return nc.dram_tensor(name, list(shape), dt, kind=kind).ap()

    x_in = din("x", [S, D])
    ctx_in = din("ctx", [CTX, D])
    cvec = din("cvecs", [128, KD, 2])
    w_mod = din("w_mod", [L, D, 6 * D])
    b_modF = din("b_modF", [L, 128, 6 * KD])
    b_mod = din("b_mod", [L, 6 * D])
    w_in = din("w_in", [L, D, INW])
    qk_norm = din("qk_norm", [L, 128, 2])
    sg_wT = din("sg_wT", [L, 128, 4, 128])
    sg_b = din("sg_b", [L, 512])
    w_branch = din("w_branch", [L, 3, 512, D])
    w_out = din("w_out", [L, D, D])
    ln_gb = din("ln_gb", [L, 4, D])
    ffn_wg = din("ffn_w_gate", [ND, D, cfg.DFF])
    ffn_wu = din("ffn_w_up", [ND, D, cfg.DFF])
    ffn_wd = din("ffn_w_down", [ND, cfg.DFF, D])
    if NM:
        router = din("router", [NM, D, NE])
        exp_wg = din("exp_w_gate", [NM, NE, D, cfg.DFFE])
        exp_wu = din("exp_w_up", [NM, NE, D, cfg.DFFE])
        exp_wd = din("exp_w_down", [NM, NE, cfg.DFFE, D])
    identb_d = din("identb", [128, 128], BF16)
    rotP_d = din("rotP", [128, 128], BF16)
    blockones_d = din("blockones", [128, 128], BF16)
    onesb_d = din("onesb", [128, 128], BF16)
    ropecs_d = din("ropecs", [2, 128, TOT])
    dftL_d = din("dftL", [NT, cfg.NKB, 128, 2, 256], BF16)
    dftC_d = din("dftC", [NCT, 1, 128, 2, CTX], BF16)
    c128_d = din("c128", [128, 2, 128], BF16)
    out_d = nc.dram_tensor("out", [S, D], F32, kind="ExternalOutput").ap()

    hT_d = dscr("hT_d", [128, KD, TOT], BF16)
    ftok_d = dscr("ftok_d", [TT, 128, 512], BF16)
    yb_d = [dscr("y%d_d" % n, [128, 4, TOT], BF16) for n in range(3)]
    x1_d = dscr("x1_d", [TOT, D], F32)
    x2_d = [dscr("x2_d%d" % l, [TOT, D], F32) for l in range(max(L - 1, 1))]

    P = Prog(nc)
    es = contextlib.ExitStack()
    with es:
        def sb(name, shape, dt):
            return es.enter_context(nc.sbuf_tensor("s_" + name, list(shape), dt))

        PB = [es.enter_context(nc.psum_tensor("pb%d" % i, [128, 512], F32)) for i in range(6)]
        PT = [es.enter_context(nc.psum_tensor("pt%d" % i, [128, 1024], BF16)) for i in range(2)]
        PTF = [PT[i].bitcast(F32) for i in range(2)]
        ring_state = {"i": 0}

        def ps_next():
            i = ring_state["i"]
            ring_state["i"] = (i + 1) % 4
            return PB[i], ("ps", i)

        pt_state = {"i": 0}

        def pt_next():
            i = pt_state["i"]
            pt_state["i"] = 1 - i
            return PT[i][:, 0:512], ("pt", i)

        class Ring:
            def __init__(self, name, n, shape, dt):
                self.t = [sb("%s%d" % (name, i), shape, dt) for i in range(n)]
                self.name = name
                self.n = n
                self.i = 0

            def next(self):
                i = self.i
                self.i = (i + 1) % self.n
                return self.t[i], (self.name, i)

        identb = sb("identb", [128, 128], BF16)
        rotP = sb("rotP", [128, 128], BF16)
        blockones = sb("blockones", [128, 128], BF16)
        selb = sb("onesb", [128, 128], BF16)
        c128 = sb("c128", [128, 2, 128], BF16)
        epsb = sb("epsb", [128, 1], F32)
        zeros = sb("zeros", [128, 128], F32)
        for nm, t, d in (("identb", identb, identb_d), ("rotP", rotP, rotP_d), ("blockones", blockones, blockones_d),
                         ("selb", selb, onesb_d), ("c128", c128, c128_d)):
            P.D("sp", [(t[:], d)], key=nm, writes=[nm])
        P.I("pool", "memset", epsb[:], 1e-6, writes=["epsb"])
        P.I("pool", "memset", zeros[:], 0.0, writes=["zeros"])

        cv32 = sb("cv32", [128, KD, 2], F32)
        scT = sb("scT", [128, KD, 2], BF16)
        sc32 = sb("sc32", [128, KD, 2], F32)
        modF = sb("modF", [128, 6 * KD, 2], F32)
        bmF = sb("bmF", [128, 6 * KD], F32)
        bcg = sb("bcg", [128, 4, D], F32)
        lngb = sb("lngb", [128, 4, D], F32)
        qkn = sb("qkn", [128, 2], F32)

        P.D("sp", [(cv32[:], cvec)], key="cv32", writes=["cv32"])
        P.I("act", "activation", sc32[:], cv32[:], AF.Silu, reads=["cv32"], writes=["sc32"])
        P.I("dve", "tensor_copy", scT[:], sc32[:], reads=["sc32"], writes=["scT"])

        ARENA_BYTES = cfg.ARENA_KB * 1024
        arena_f = sb("arena", [128, ARENA_BYTES // 4], F32)
        arena_b = arena_f.bitcast(BF16)
        ar = {"off": 0}
        R = {}
        T = {}

        def a_alloc(shape, dt):
            n = 1
            for d_ in shape[1:]:
                n *= d_
            esz = 4 if dt == F32 else 2
            off = ar["off"]
            nbytes = (n * esz + 63) // 64 * 64
            assert off + nbytes <= ARENA_BYTES, ("arena overflow", off, nbytes, ARENA_BYTES)
            ar["off"] = off + nbytes
            base = arena_f if dt == F32 else arena_b
            v = base[:, off // esz:off // esz + n]
            if len(shape) == 3:
                v = v.rearrange("p (a b) -> p a b", a=shape[1])
            elif len(shape) == 4:
                v = v.rearrange("p (a b c) -> p a b c", a=shape[1], b=shape[2])
            return v

        class Ring:
            def __init__(self, name, n, shape, dt):
                self.t = [a_alloc(shape, dt) for i in range(n)]
                self.name = name
                self.n = n
                self.i = 0

            def next(self):
                i = self.i
                self.i = (i + 1) % self.n
                return self.t[i], (self.name, i)

        SPEC = {
            "wm": (2, [128, KD, 512], BF16), "xt": (2, [128, D], F32), "xn4": (2, [128, 4, D], BF16),
            "hTb": (2, [128, KD, 512], BF16), "stt": (4, [128, 8, 6], F32), "mv": (4, [128, 4, 2], F32),
            "sd": (4, [128, 4], F32), "rs": (4, [128, 4], F32),
            "tmpA": (2, [128, 512], F32), "tmpB": (2, [128, 512], F32), "tmpC": (2, [128, 512], F32),
            "tbf": (2, [128, 512], BF16), "tbf2": (2, [128, 512], BF16), "rcs": (2, [128, 2, 512], F32),
            "r": (2, [128, D], F32), "xo": (2, [128, D], F32),
            "wgu": (2, [128, KD, 2, cfg.SL * 128], BF16), "wd": (2, [128, cfg.SL, D], BF16), "actT": (2, [128, cfg.SL, 512], BF16),
            "yblk": (2, [128, 4, 512], BF16), "qT": (8, [128, 512], BF16), "pT": (4, [128, 512], BF16),
            "mx": (2, [128, 4, 512], F32), "gv": (2, [128, 512], F32), "vg": (2, [128, 512], BF16),
            "dft": (4, [128, 2, 256], BF16), "yfr": (2, [128, 4, 256], BF16),
            "mT": (1, [128, KD, 512], BF16), "ybs0": (1, [128, 4, 512], BF16), "ybs1": (1, [128, 4, 512], BF16),
            "ybs2": (1, [128, 4, 512], BF16), "acc": (2, [128, 512], F32),
        }

        def make_ring(nm):
            cnt = None
            if isinstance(nm, tuple):
                nm, cnt = nm
            n_, shp, dt = SPEC[nm]
            R[nm] = Ring(nm, cnt or n_, shp, dt)

        def phase(rings=(), tensors=(), keep=0):
            P.barrier()
            ar["off"] = keep
            for k_ in list(R.keys()):
                del R[k_]
            for nm, shp, dt in tensors:
                T[nm] = a_alloc(shp, dt)
            for nm in rings:
                make_ring(nm)

        LN_RINGS = ("stt", "mv", "sd", "rs")
        HT_RINGS = (("xt", 4), "xn4", "hTb") + LN_RINGS

        NF = D // 512

        def mm(out_ap, pairs, reads, writes):
            pairs = list(pairs)
            n_ = len(pairs)
            P.M("pe", [("matmul", (out_ap, a, b), dict(start=(i == 0), stop=(i == n_ - 1))) for i, (a, b) in enumerate(pairs)],
                reads, writes)

        def ln_multi(items):
            bufs = []
            for it in items:
                bufs.append((R["stt"].next(), R["mv"].next(), R["sd"].next(), R["rs"].next()))
            for (src_ap, src_tok, dst_ap, dst_tok), ((st, stt), (mv, mvt), (sd, sdt), (rs, rst)) in zip(items, bufs):
                P.M("dve", [("bn_stats", (st[:, f, :], src_ap[:, f * 512:(f + 1) * 512]), {}) for f in range(NF)], reads=[src_tok], writes=[stt])
            for (src_ap, src_tok, dst_ap, dst_tok), ((st, stt), (mv, mvt), (sd, sdt), (rs, rst)) in zip(items, bufs):
                P.I("dve", "bn_aggr", mv[:, 0, :], st[:, 0:NF, :].rearrange("p g s -> p (g s)"), reads=[stt], writes=[mvt])
            for (src_ap, src_tok, dst_ap, dst_tok), ((st, stt), (mv, mvt), (sd, sdt), (rs, rst)) in zip(items, bufs):
                P.I("act", "activation", sd[:, 0:1], mv[:, 0, 1:2], AF.Sqrt, bias=epsb[:], reads=[mvt, "epsb"], writes=[sdt])
            for (src_ap, src_tok, dst_ap, dst_tok), ((st, stt), (mv, mvt), (sd, sdt), (rs, rst)) in zip(items, bufs):
                P.I("dve", "reciprocal", rs[:, 0:1], sd[:, 0:1], reads=[sdt], writes=[rst])
            for (src_ap, src_tok, dst_ap, dst_tok), ((st, stt), (mv, mvt), (sd, sdt), (rs, rst)) in zip(items, bufs):
                P.I("dve", "tensor_scalar", dst_ap, src_ap, mv[:, 0, 0:1], rs[:, 0:1], ALU.subtract, ALU.mult,
                    reads=[src_tok, mvt, rst], writes=[dst_tok])

        def x_src(l, tile):
            if l == 0:
                if tile < NT:
                    return x_in[tile * 128:(tile + 1) * 128, :], None
                return ctx_in[(tile - NT) * 128:(tile - NT + 1) * 128, :], None
            return x2_d[l - 1][tile * 128:(tile + 1) * 128, :], ("x2_d", l - 1, tile)

        def make_hT(blk, src_fn, shift_j, scale_j):
            t0, n, s_ = blk
            nt = n // 128
            xn, xnt = R["xn4"].next()
            items = []
            for ti in range(nt):
                tile = t0 // 128 + ti
                src, srctok = src_fn(tile)
                xt, xtt = R["xt"].next()
                P.D("sp", [(xt[:], src)], key=xtt, reads=[srctok] if srctok else [], writes=[xtt])
                items.append((xt[:], xtt, xn[:, ti, :], (xnt, ti)))
            ln_multi(items)
            hTb, hTt = R["hTb"].next()
            for k in range(KD):
                pt, ptt = pt_next()
                P.M("pe", [("transpose", (pt[:, ti * 128:(ti + 1) * 128], xn[:, ti, k * 128:(k + 1) * 128], identb[:]), {}) for ti in range(nt)],
                    reads=[(xnt, ti) for ti in range(nt)] + ["identb"], writes=[ptt])
                P.I("dve", "tensor_scalar", hTb[:, k, 0:n], pt[:, 0:n],
                    modF[:, scale_j + k, s_:s_ + 1], modF[:, shift_j + k, s_:s_ + 1], ALU.mult, ALU.add,
                    reads=[ptt, "modF"], writes=[(hTt, k)])
            return hTb, [(hTt, k) for k in range(KD)]

        def gelu(src_ps, src_tok, dst_ap, dst_tok, n, extra_mul=None, extra_toks=()):
            a, at = R["tmpA"].next()
            b, bt = R["tmpB"].next()
            P.I("act", "activation", a[:, 0:n], src_ps, AF.Square, reads=[src_tok], writes=[at])
            P.I("dve", "tensor_scalar", a[:, 0:n], a[:, 0:n], 0.044715, 1.0, ALU.mult, ALU.add, reads=[at], writes=[at])
            P.I("dve", "tensor_tensor", a[:, 0:n], a[:, 0:n], src_ps, ALU.mult, reads=[at, src_tok], writes=[at])
            P.I("act", "activation", b[:, 0:n], a[:, 0:n], AF.Sigmoid, scale=1.5957691216057308, reads=[at], writes=[bt])
            if extra_mul is None:
                P.I("dve", "tensor_tensor", dst_ap, b[:, 0:n], src_ps, ALU.mult, reads=[bt, src_tok], writes=[dst_tok])
            else:
                P.I("dve", "tensor_tensor", b[:, 0:n], b[:, 0:n], src_ps, ALU.mult, reads=[bt, src_tok], writes=[bt])
                P.I("dve", "tensor_tensor", dst_ap, b[:, 0:n], extra_mul, ALU.mult, reads=[bt] + list(extra_toks), writes=[dst_tok])

        def rope_norm(zps, ztok, gcol, t0, n, dst_ap, dst_tok):
            sq, sqt = R["tbf"].next()
            zg, zgt = R["tbf2"].next()
            P.I("act", "activation", sq[:, 0:n], zps, AF.Square, reads=[ztok], writes=[sqt])
            P.I("dve", "tensor_scalar", zg[:, 0:n], zps, qkn[:, gcol:gcol + 1], None, ALU.mult, reads=[ztok, "qkn"], writes=[zgt])
            pa, pat = ps_next()
            pb, pbt = ps_next()
            mm(pa[:, 0:n], [(blockones[:], sq[:, 0:n])], reads=[sqt, "blockones"], writes=[pat])
            mm(pb[:, 0:n], [(rotP[:], zg[:, 0:n])], reads=[zgt, "rotP"], writes=[pbt])
            cs, cst = R["rcs"].next()
            P.D("sp", [(cs[:, c_, 0:n], ropecs_d[c_, :, t0:t0 + n]) for c_ in range(2)], key=cst, writes=[cst])
            a, at = R["tmpA"].next()
            b, bt = R["tmpB"].next()
            c2, ct = R["tmpC"].next()
            P.I("act", "activation", a[:, 0:n], pa[:, 0:n], AF.Sqrt, bias=epsb[:], scale=1.0 / 64.0, reads=[pat, "epsb"], writes=[at])
            P.I("dve", "reciprocal", a[:, 0:n], a[:, 0:n], reads=[at], writes=[at])
            P.I("dve", "tensor_tensor", b[:, 0:n], zg[:, 0:n], cs[:, 0, 0:n], ALU.mult, reads=[zgt, cst], writes=[bt])
            P.I("dve", "tensor_tensor", c2[:, 0:n], pb[:, 0:n], cs[:, 1, 0:n], ALU.mult, reads=[pbt, cst], writes=[ct])
            P.I("dve", "tensor_tensor", b[:, 0:n], b[:, 0:n], c2[:, 0:n], ALU.add, reads=[bt, ct], writes=[bt])
            P.I("dve", "tensor_tensor", dst_ap, b[:, 0:n], a[:, 0:n], ALU.mult, reads=[bt, at], writes=[dst_tok])

        def wload(dst, dtok, src_ap, key):
            P.D("pool", [(dst, src_ap)], key=key, writes=[dtok])

        def post_norm_multi(tiles, gate_idx, g_idx, b_idx):
            st = []
            for (y_fn, dst_ap, dst_tok_d, src_ap, src_tok_d) in tiles:
                r, rt = R["r"].next()
                xt, xtt = R["xt"].next()
                xo, xot = R["xo"].next()
                P.D("sp", [(xt[:], src_ap)], key=xtt, reads=[src_tok_d] if src_tok_d else [], writes=[xtt])
                st.append((r, rt, xt, xtt, xo, xot))
            for (y_fn, dst_ap, dst_tok_d, src_ap, src_tok_d), (r, rt, xt, xtt, xo, xot) in zip(tiles, st):
                for cb in range(NF):
                    yap, ytok = y_fn(cb)
                    P.I("dve", "tensor_tensor", r[:, cb * 512:(cb + 1) * 512], yap, bcg[:, gate_idx, cb * 512:(cb + 1) * 512], ALU.mult,
                        reads=[ytok, "bcg"], writes=[(rt, cb)])
            for (r, rt, xt, xtt, xo, xot) in st:
                P.I("dve", "scalar_tensor_tensor", r[:], xt[:], cfg.ALPHA, r[:], ALU.mult, ALU.add,
                    reads=[xtt] + [(rt, cb) for cb in range(NF)], writes=[rt])
            ln_multi([(r[:], rt, xo[:], xot) for (r, rt, xt, xtt, xo, xot) in st])
            for (r, rt, xt, xtt, xo, xot) in st:
                P.I("dve", "tensor_tensor", xo[:], xo[:], lngb[:, g_idx, :], ALU.mult, reads=[xot, "lngb"], writes=[xot])
            for (r, rt, xt, xtt, xo, xot) in st:
                P.I("pool", "tensor_tensor", xo[:], xo[:], lngb[:, b_idx, :], ALU.add, reads=[xot, "lngb"], writes=[xot])
            for (y_fn, dst_ap, dst_tok_d, src_ap, src_tok_d), (r, rt, xt, xtt, xo, xot) in zip(tiles, st):
                P.D("sp", [(dst_ap, xo[:])], key=xot, reads=[xot], writes=[dst_tok_d])

        lat_blocks = [(b * 512, 512, 0) for b in range(S // 512)]
        ctx_block = (S, CTX, 1)

        SL = cfg.SL
        HALF_T = max(NT // 2, NCT)
        all_k = [("kT", h, b[0]) for h in range(2) for b in lat_blocks + [ctx_block]]

        def load_hT(blk):
            t0, n, s_ = blk
            hTb, hTt = R["hTb"].next()
            P.D("sp", [(hTb[:, :, 0:n], hT_d[:, :, t0:t0 + n])], key=("hTld", hTt),
                reads=[("hT_d", t0)], writes=[(hTt, k) for k in range(KD)])
            return hTb, [(hTt, k) for k in range(KD)]

        def modulation(l):
            scB, bmbc = T["scB"], T["bmbc"]
            for s_ in range(2):
                for k in range(KD):
                    P.I("dve", "tensor_scalar", scB[:, s_, k, :], zeros[:], sc32[:, k, s_:s_ + 1], None, ALU.add,
                        reads=["zeros", "sc32"], writes=[("scB", s_, k)])
            P.D("sp", [(bmF[:], b_modF[l])], key="bmF", writes=["bmF"])
            P.D("sp", [(lngb[:], ln_gb[l:l + 1].broadcast_to([128, 4, D]))], key="lngb", writes=["lngb"])
            P.D("sp", [(bmbc[:, 0, :], b_mod[l:l + 1, 2 * D:3 * D].broadcast_to([128, D])),
                       (bmbc[:, 1, :], b_mod[l:l + 1, 5 * D:6 * D].broadcast_to([128, D]))], key="bmbc", writes=["bmbc"])
            P.D("sp", [(qkn[:], qk_norm[l])], key="qkn", writes=["qkn"])
            wmv = w_mod[l].rearrange("(k p) n -> p k n", p=128)
            nslab = 6 * D // 512
            for sl in range(nslab):
                wm, wmt = R["wm"].next()
                wload(wm[:], wmt, wmv[:, :, sl * 512:(sl + 1) * 512], key=wmt)
                part = (sl * 512) // D
                if part in (2, 5):
                    gi = 0 if part == 2 else 1
                    col0 = sl * 512 - part * D
                    for s_ in range(2):
                        pb, pbt = ps_next()
                        mm(pb[:], [(scB[:, s_, k, :], wm[:, k, :]) for k in range(KD)],
                           reads=[wmt] + [("scB", s_, k) for k in range(KD)], writes=[pbt])
                        P.I("dve", "tensor_tensor", bcg[:, gi * 2 + s_, col0:col0 + 512], pb[:], bmbc[:, gi, col0:col0 + 512], ALU.add,
                            reads=[pbt, "bmbc"], writes=["bcg"])
                else:
                    pb, pbt = ps_next()
                    j0 = sl * 4
                    P.M("pe", [("matmul", (pb[:, jj * 2:jj * 2 + 2], wm[:, k, jj * 128:(jj + 1) * 128], scT[:, k, :]),
                                dict(start=(k == 0), stop=(k == KD - 1))) for jj in range(4) for k in range(KD)],
                        reads=[wmt, "scT"], writes=[pbt])
                    for s_ in range(2):
                        P.I("dve", "tensor_tensor", modF[:, j0:j0 + 4, s_],
                            pb[:, 0:8].rearrange("p (j s) -> p j s", s=2)[:, :, s_], bmF[:, j0:j0 + 4], ALU.add,
                            reads=[pbt, "bmF"], writes=["modF"])
            for base in (KD, 4 * KD):
                P.I("dve", "tensor_scalar", modF[:, base:base + KD, :], modF[:, base:base + KD, :], 1.0, None, ALU.add,
                    reads=["modF"], writes=["modF"])

        def phase_A_block(l, blk, wi, ctx_out):
            kT, V3, wA = T["kT"], T["V3"], T["wA"]
            wkd = wA[:, :, 0:256].rearrange("p k (h c) -> p k h c", h=2)
            wv = wA[:, :, 256:384]
            wft = wA[:, :, 512:1024]
            t0, n, s_ = blk
            nt = n // 128
            hTb, hTtoks = make_hT(blk, (lambda tile: x_src(l, tile)), 0, KD)
            P.D("sp", [(hT_d[:, :, t0:t0 + n], hTb[:, :, 0:n])], key=("hTst", hTtoks[0][0]), reads=hTtoks, writes=[("hT_d", t0)])
            for h in range(2):
                pz, pzt = ps_next()
                mm(pz[:, 0:n], [(wkd[:, k, h, :], hTb[:, k, 0:n]) for k in range(KD)], reads=hTtoks + ["wA"], writes=[pzt])
                rope_norm(pz[:, 0:n], pzt, 1, t0, n, kT[:, h, t0:t0 + n], ("kT", h, t0))
            for ti in range(nt):
                tile = t0 // 128 + ti
                pz, pzt = ps_next()
                mm(pz[:, 0:128], [(hTb[:, k, ti * 128:(ti + 1) * 128], wv[:, k, :]) for k in range(KD)], reads=hTtoks + ["wA"], writes=[pzt])
                pzv = pz[:, 0:128].rearrange("p (h c) -> p h c", h=2)
                P.I("act", "activation", V3[:, tile, :, 0:64], pzv, AF.Copy, reads=[pzt], writes=[("V3a", tile)])
                P.I("pool", "tensor_copy", V3[:, tile, :, 128:192], V3[:, tile, :, 0:64], reads=[("V3a", tile)], writes=[("V3b", tile)])
                if s_ == 0 or ctx_out:
                    pf, pft = ps_next()
                    mm(pf[:], [(hTb[:, k, ti * 128:(ti + 1) * 128], wft[:, k, :]) for k in range(KD)], reads=hTtoks + ["wA"], writes=[pft])
                    fb, fbt = R["tbf"].next()
                    P.I("act", "activation", fb[:], pf[:], AF.Copy, reads=[pft], writes=[fbt])
                    P.D("sp", [(ftok_d[tile], fb[:])], key=fbt, reads=[fbt], writes=[("ftok_d", tile)])

        def attn_prologue(blk):
            wq = T["wA"]
            t0, n, s_ = blk
            hTb, hTtoks = load_hT(blk)
            qs = []
            for c in range(4):
                pz, pzt = ps_next()
                mm(pz[:, 0:n], [(wq[:, k, c * 128:(c + 1) * 128], hTb[:, k, 0:n]) for k in range(KD)], reads=hTtoks + ["wA"], writes=[pzt])
                qT, qTt = R["qT"].next()
                rope_norm(pz[:, 0:n], pzt, 0, t0, n, qT[:, 0:n], qTt)
                qs.append((qT, qTt))
            return qs

        def attn_block(blk, qs, next_blk):
            kT, V3, rsum, bcs, rhl, sAB = T["kT"], T["V3"], T["rsum"], T["bcs"], T["rhl"], T["sAB"]
            t0, n, s_ = blk
            yb, ybt = R["yblk"].next()
            key_tiles = list(range(NT, TT)) + (list(range(NT)) if s_ == 0 else [])
            nk = len(key_tiles)
            poA, poAt, poB, poBt, pbc, pbct = PB[4], "po", PB[5], "pbc", PTF[0], ("pt", 0)
            pending = {}
            next_qs = [None]

            def emit_sT(c, ki):
                qT, qTt = qs[c]
                kv = c // 2
                kt = key_tiles[ki]
                pa, pat = ps_next()
                pb, pbt = ps_next()
                P.M("pe", [("matmul", (pa[:, 0:n], kT[0:64, kv, kt * 128:(kt + 1) * 128], qT[0:64, 0:n]), dict(start=True, stop=True)),
                           ("matmul", (pb[:, 0:n], kT[64:128, kv, kt * 128:(kt + 1) * 128], qT[64:128, 0:n]), dict(start=True, stop=True))],
                    reads=[qTt] + all_k, writes=[pat, pbt])
                pending[(c, ki)] = (pa, pat, pb, pbt)

            def fin_copy(c):
                P.I("dve", "tensor_copy", sAB[:, 0, 0:n], poA[:, 0:n], reads=[poAt], writes=[("sAB", 0)])
                P.I("dve", "tensor_copy", sAB[:, 1, 0:n], poB[:, 0:n], reads=[poBt], writes=[("sAB", 1)])
                P.I("dve", "reciprocal", rsum[64:128, 0:n], sAB[64:128, 0, 0:n], reads=[("sAB", 0)], writes=[("rsum", 0)])
                P.I("dve", "reciprocal", rsum[0:64, 0:n], sAB[0:64, 1, 0:n], reads=[("sAB", 1)], writes=[("rsum", 1)])
                P.I("dve", "tensor_copy", rhl[:, 0, 0:n], rsum[:, 0:n], reads=[("rsum", 0), ("rsum", 1)], writes=["rhi"])
                P.I("dve", "tensor_tensor", rhl[:, 1, 0:n], rsum[:, 0:n], rhl[:, 0, 0:n], ALU.subtract,
                    reads=[("rsum", 0), ("rsum", 1), "rhi"], writes=["rlo"])

            def fin_norm(c):
                P.M("pe", [("matmul", (pbc[:, 0:n], selb[:], rhl[:, 0, 0:n]), dict(start=True, stop=False)),
                           ("matmul", (pbc[:, 0:n], selb[:], rhl[:, 1, 0:n]), dict(start=False, stop=True))],
                    reads=["rhi", "rlo", "selb"], writes=[pbct])
                P.I("dve", "tensor_tensor", yb[0:64, c, 0:n], sAB[0:64, 0, 0:n], pbc[0:64, 0:n], ALU.mult,
                    reads=[("sAB", 0), pbct], writes=[(ybt, c, 0)])
                P.I("dve", "tensor_tensor", yb[64:128, c, 0:n], sAB[64:128, 1, 0:n], pbc[64:128, 0:n], ALU.mult,
                    reads=[("sAB", 1), pbct], writes=[(ybt, c, 1)])

            DEFER = 14
            emit_sT(0, 0)
            for c in range(4):
                kv = c // 2
                for ki in range(nk):
                    kt = key_tiles[ki]
                    pa, pat, pb, pbt = pending.pop((c, ki))
                    pTa, pTat = R["pT"].next()
                    pTb, pTbt = R["pT"].next()
                    P.I("act", "activation", pTa[:, 0:n], pa[:, 0:n], AF.Exp, scale=0.125, reads=[pat], writes=[pTat])
                    P.I("act", "activation", pTb[:, 0:n], pb[:, 0:n], AF.Exp, scale=0.125, reads=[pbt], writes=[pTbt])
                    if ki + 1 < nk:
                        emit_sT(c, ki + 1)
                    elif c + 1 < 4:
                        if c + 1 == 2 and next_blk is not None:
                            next_qs[0] = attn_prologue(next_blk)
                        emit_sT(c + 1, 0)
                    P.M("pe", [("matmul", (poA[:, 0:n], V3[:, kt, kv, 0:128], pTa[:, 0:n]), dict(start=(ki == 0), stop=(ki == nk - 1))),
                               ("matmul", (poB[:, 0:n], V3[:, kt, kv, 64:192], pTb[:, 0:n]), dict(start=(ki == 0), stop=(ki == nk - 1)))],
                        reads=[pTat, pTbt, ("V3a", kt), ("V3b", kt), "V3ones"], writes=[poAt, poBt])
                    if c > 0 and ki == min(DEFER, nk - 1):
                        fin_norm(c - 1)
                if c == 3:
                    fin_copy(c)
                    fin_norm(c)
                else:
                    fin_copy(c)
            P.D("sp", [(yb_d[0][:, :, t0:t0 + n], yb[:, :, 0:n])], key=("yst", ybt),
                reads=[(ybt, c, h_) for c in range(4) for h_ in range(2)], writes=[("y_d", 0, t0)])
            return next_qs[0]

        def sg_block(blk):
            wA, wB, sgbias = T["wA"], T["wB"], T["sgbias"]
            wsgv = wA[:, :, 0:512]
            wu_ = wA[:, :, 512:1024]
            wsT = wB
            t0, n, s_ = blk
            nt = n // 128
            hTb, hTtoks = load_hT(blk)
            mx, mxt = R["mx"].next()
            for ti in range(nt):
                pz, pzt = ps_next()
                mm(pz[:], [(hTb[:, k, ti * 128:(ti + 1) * 128], wsgv[:, k, :]) for k in range(KD)], reads=hTtoks + ["wA"], writes=[pzt])
                gv, gvt = R["gv"].next()
                gelu(pz[:], pzt, gv[:], gvt, 512)
                st, stt = R["stt"].next()
                mv, mvt = R["mv"].next()
                sd, sdt = R["sd"].next()
                rs, rst = R["rs"].next()
                P.M("dve", [("bn_stats", (st[:, g, :], gv[:, g * 128:(g + 1) * 128]), {}) for g in range(4)], reads=[gvt], writes=[stt])
                P.M("dve", [("bn_aggr", (mv[:, g, :], st[:, g, :]), {}) for g in range(4)], reads=[stt], writes=[mvt])
                P.I("act", "activation", sd[:], mv[:, :, 1], AF.Sqrt, bias=epsb[:], reads=[mvt, "epsb"], writes=[sdt])
                P.I("dve", "reciprocal", rs[:], sd[:], reads=[sdt], writes=[rst])
                vg, vgt = R["vg"].next()
                for g in range(4):
                    P.I("dve", "tensor_scalar", vg[:, g * 128:(g + 1) * 128], gv[:, g * 128:(g + 1) * 128], mv[:, g, 0:1], rs[:, g:g + 1],
                        ALU.subtract, ALU.mult, reads=[gvt, mvt, rst], writes=[(vgt, g)])
                pm, pmt = ps_next()
                P.M("pe", [("matmul", (pm[:, g * 128:(g + 1) * 128], vg[:, g * 128:(g + 1) * 128], wsT[:, g, :]), dict(start=True, stop=True))
                           for g in range(4)], reads=[(vgt, g) for g in range(4)] + ["wB"], writes=[pmt])
                P.I("dve", "tensor_tensor", mx[:, :, ti * 128:(ti + 1) * 128], pm[:].rearrange("p (g c) -> p g c", g=4),
                    sgbias[:].rearrange("p (g c) -> p g c", g=4), ALU.add, reads=[pmt, "sgbias"], writes=[(mxt, ti)])
            yb, ybt = R["yblk"].next()
            for g in range(4):
                pu, put = ps_next()
                mm(pu[:, 0:n], [(wu_[:, k, g * 128:(g + 1) * 128], hTb[:, k, 0:n]) for k in range(KD)], reads=hTtoks + ["wA"], writes=[put])
                gelu(pu[:, 0:n], put, yb[:, g, 0:n], (ybt, g), n, extra_mul=mx[:, g, 0:n], extra_toks=[(mxt, ti) for ti in range(nt)])
            P.D("sp", [(yb_d[1][:, :, t0:t0 + n], yb[:, :, 0:n])], key=("yst", ybt),
                reads=[(ybt, g) for g in range(4)], writes=[("y_d", 1, t0)])

        def fourier(tile0, ntile, table, nkb, kw, tokbase):
            ftok_sb, Gsb = T["ftok_sb"], T["Gsb"]
            Ltot = ntile * 128
            scale = float(1.0 / np.sqrt(Ltot * 128.0))
            P.D("sp", [(ftok_sb[:, i, :], ftok_d[tile0 + i]) for i in range(ntile)],
                key="ftok_sb", reads=[("ftok_d", tile0 + i) for i in range(ntile)], writes=["ftok_sb"])
            for kb in range(nkb):
                for lc in range(ntile):
                    dt_, dtt = R["dft"].next()
                    P.D("sp", [(dt_[:, :, 0:kw], table[lc, kb])], key=dtt, writes=[dtt])
                    P.M("pe", [("matmul", (PB[g][:, cs_ * 256:cs_ * 256 + kw], ftok_sb[:, lc, g * 128:(g + 1) * 128], dt_[:, cs_, 0:kw]),
                                dict(start=(lc == 0 and cs_ == 0), stop=(lc == ntile - 1), skip_group_check=True)) for g in range(4) for cs_ in range(2)],
                        reads=[dtt, "ftok_sb"], writes=[("ps", g) for g in range(4)])
                for g in range(4):
                    src = PB[g][:].rearrange("p (c k) -> p c k", c=2)[:, :, 0:kw]
                    if g % 2 == 0:
                        P.I("act", "activation", Gsb[:, g, :, 0:kw], src, AF.Copy, reads=[("ps", g)], writes=[("Gsb", g)])
                    else:
                        P.I("dve", "tensor_copy", Gsb[:, g, :, 0:kw], src, reads=[("ps", g)], writes=[("Gsb", g)])
                yf, yft = R["yfr"].next()
                for g in range(4):
                    po_ = PB[4] if g % 2 == 0 else PB[5]
                    pot = "po" if g % 2 == 0 else "pbc"
                    mm(po_[:, 0:kw], [(c128[:, 0, :], Gsb[:, g, 0, 0:kw]), (c128[:, 1, :], Gsb[:, g, 1, 0:kw])],
                       reads=[("Gsb", g), "c128"], writes=[pot])
                    P.I("act", "activation", yf[:, g, 0:kw], po_[:, 0:kw], AF.Copy, scale=scale, reads=[pot], writes=[(yft, g)])
                tb = tokbase + kb * kw
                P.D("sp", [(yb_d[2][:, :, tb:tb + kw], yf[:, :, 0:kw])], key=("yfst", yft),
                    reads=[(yft, g) for g in range(4)], writes=[("y_d", 2, tb)])

        def merge_loads(blk):
            t0, n, s_ = blk
            hTb, hTtoks = load_hT(blk)
            ys = []
            for n_ in range(3):
                yb, ybt = R["ybs%d" % n_].next()
                if n_ == 2:
                    rd = [("y_d", 2, t0 + i * 256) for i in range(n // 256)] if s_ == 0 else [("y_d", 2, t0)]
                else:
                    rd = [("y_d", n_, t0)]
                P.D("sp", [(yb[:, :, 0:n], yb_d[n_][:, :, t0:t0 + n])], key=("ybld", ybt), reads=rd, writes=[ybt])
                ys.append((yb, ybt))
            return hTb, hTtoks, ys

        def merge_block(l, blk, loaded, next_blk):
            wA, wB, wC = T["wA"], T["wB"], T["wC"]
            wgate = wA
            t0, n, s_ = blk
            nt = n // 128
            hTb, hTtoks, ys = loaded
            mT, mTt = R["mT"].next()
            for dc in range(KD):
                acc, acct = R["acc"].next()
                for n_ in range(3):
                    yb, ybt = ys[n_]
                    pp, ppt = ps_next()
                    mm(pp[:, 0:n], [(wB[:, n_, c, dc * 128:(dc + 1) * 128], yb[:, c, 0:n]) for c in range(4)], reads=[ybt, "wB"], writes=[ppt])
                    pg, pgt = ps_next()
                    mm(pg[:, 0:n], [(wgate[:, k, n_ * D + dc * 128:n_ * D + (dc + 1) * 128], hTb[:, k, 0:n]) for k in range(KD)],
                       reads=hTtoks + ["wA"], writes=[pgt])
                    sg, sgt = R["tmpA"].next()
                    P.I("act", "activation", sg[:, 0:n], pg[:, 0:n], AF.Sigmoid, reads=[pgt], writes=[sgt])
                    if n_ == 0:
                        P.I("dve", "tensor_tensor", acc[:, 0:n], sg[:, 0:n], pp[:, 0:n], ALU.mult, reads=[sgt, ppt], writes=[acct])
                    else:
                        P.I("dve", "tensor_tensor", sg[:, 0:n], sg[:, 0:n], pp[:, 0:n], ALU.mult, reads=[sgt, ppt], writes=[sgt])
                        if n_ == 1:
                            P.I("pool", "tensor_tensor", acc[:, 0:n], acc[:, 0:n], sg[:, 0:n], ALU.add, reads=[sgt, acct], writes=[acct])
                        else:
                            P.I("dve", "tensor_tensor", mT[:, dc, 0:n], acc[:, 0:n], sg[:, 0:n], ALU.add, reads=[sgt, acct], writes=[(mTt, dc)])
            nxt = merge_loads(next_blk) if next_blk is not None else None
            for tg in range(0, nt, 2):
                grp = []
                for ti in range(tg, min(tg + 2, nt)):
                    tile = t0 // 128 + ti
                    pys = []
                    for cb in range(NF):
                        py, pyt = ps_next()
                        mm(py[:], [(mT[:, dc, ti * 128:(ti + 1) * 128], wC[:, dc, cb * 512:(cb + 1) * 512]) for dc in range(KD)],
                           reads=[(mTt, dc) for dc in range(KD)] + ["wC"], writes=[pyt])
                        pys.append((py[:], pyt))
                    src, srctok = x_src(l, tile)
                    grp.append(((lambda cb, pys=pys: pys[cb]), x1_d[tile * 128:(tile + 1) * 128, :], ("x1_d", tile), src, srctok))
                post_norm_multi(grp, 0 + s_, 0, 1)
            return nxt

        def router_tile(hTb, hTtoks, ti, lt):
            wr, gates = T["wr"], T["gates"]
            pr, prt = ps_next()
            mm(pr[:, 0:NE], [(hTb[:, k, ti * 128:(ti + 1) * 128], wr[:, k, 0:NE]) for k in range(KD)], reads=hTtoks + ["wr"], writes=[prt])
            lg, lgt = R["tmpA"].next()
            m8, m8t = R["tmpB"].next()
            P.I("dve", "tensor_copy", lg[:, 0:8], pr[:, 0:8], reads=[prt], writes=[lgt])
            P.I("dve", "max", m8[:, 0:8], lg[:, 0:8], reads=[lgt], writes=[m8t])
            P.I("dve", "tensor_tensor", m8[:, 8:9], m8[:, 0:1], m8[:, 1:2], ALU.subtract, reads=[m8t], writes=[(m8t, "d")])
            P.I("act", "activation", m8[:, 9:10], m8[:, 8:9], AF.Sigmoid, reads=[(m8t, "d")], writes=[(m8t, "w1")])
            P.I("act", "activation", m8[:, 10:11], m8[:, 8:9], AF.Sigmoid, scale=-1.0, reads=[(m8t, "d")], writes=[(m8t, "w2")])
            P.I("dve", "tensor_scalar", lg[:, 8:16], lg[:, 0:8], m8[:, 0:1], m8[:, 9:10], ALU.is_equal, ALU.mult,
                reads=[lgt, m8t, (m8t, "w1")], writes=[(lgt, "g1")])
            P.I("dve", "tensor_scalar", lg[:, 16:24], lg[:, 0:8], m8[:, 1:2], m8[:, 10:11], ALU.is_equal, ALU.mult,
                reads=[lgt, m8t, (m8t, "w2")], writes=[(lgt, "g2")])
            P.I("dve", "tensor_tensor", gates[:, lt, :], lg[:, 8:16], lg[:, 16:24], ALU.add,
                reads=[(lgt, "g1"), (lgt, "g2")], writes=[("gates", lt)])

        def ffn(l, ctx_out, last):
            moe = (l % 2 == 1)
            li = l // 2
            if moe:
                nexp, dff = NE, cfg.DFFE
            else:
                nexp, dff = 1, cfg.DFF
            nch = dff // 128
            slabs = [(c0, min(SL, nch - c0)) for c0 in range(0, nch, SL)]
            half_tiles = NT // 2
            bph = half_tiles // 4
            tok_sets = [[(b_ * 512, 512, 0) for b_ in range(h_ * bph, (h_ + 1) * bph)] for h_ in range(2)]
            if ctx_out:
                tok_sets.append([ctx_block])
            fixed = [("yacc", [128, HALF_T, D], F32), ("hT2", [128, KD, HALF_T * 128], BF16),
                     ("gates", [128, HALF_T, 8], F32), ("wr", [128, KD, 8], BF16)]
            for tset in tok_sets:
                base_tok = tset[0][0]
                phase(HT_RINGS + ("tmpA", "tmpB"), fixed)
                keep = ar["off"] if False else None
                yacc, hT2, gates, wr = T["yacc"], T["hT2"], T["gates"], T["wr"]
                fixed_bytes = sum(((int(np.prod(shp[1:])) * (4 if dt == F32 else 2) + 63) // 64 * 64) for _, shp, dt in fixed)
                P.barrier()
                ar["off"] = 0
                for nm, shp, dt in fixed:
                    T[nm] = a_alloc(shp, dt)
                yacc, hT2, gates, wr = T["yacc"], T["hT2"], T["gates"], T["wr"]
                for k_ in list(R.keys()):
                    del R[k_]
                for nm in HT_RINGS + ("tmpA", "tmpB"):
                    make_ring(nm)
                if moe:
                    wload(wr[:, :, 0:NE], "wr", router[li].rearrange("(k p) e -> p k e", p=128), key="wr")
                for blk in tset:
                    t0, n, s_ = blk
                    nt = n // 128
                    hTb, hTtoks = make_hT(blk, (lambda tile: (x1_d[tile * 128:(tile + 1) * 128, :], ("x1_d", tile))), 3 * KD, 4 * KD)
                    off = t0 - base_tok
                    P.I("pool", "tensor_copy", hT2[:, :, off:off + n], hTb[:, :, 0:n], reads=hTtoks, writes=[("hT2", t0)])
                    if moe:
                        for ti in range(nt):
                            router_tile(hTb, hTtoks, ti, off // 128 + ti)
                P.barrier()
                ar["off"] = fixed_bytes
                for k_ in list(R.keys()):
                    del R[k_]
                for nm in ("wgu", "wd", "actT", "tmpC"):
                    make_ring(nm)
                first = True
                for e_ in range(nexp):
                    if moe:
                        wg_, wu2, wd2 = exp_wg[li, e_], exp_wu[li, e_], exp_wd[li, e_]
                    else:
                        wg_, wu2, wd2 = ffn_wg[li], ffn_wu[li], ffn_wd[li]
                    wgv = wg_.rearrange("(k p) f -> p k f", p=128)
                    wuv = wu2.rearrange("(k p) f -> p k f", p=128)
                    wdv = wd2.rearrange("(c p) d -> p c d", p=128)
                    for (c0, ncs) in slabs:
                        wgu, wgut = R["wgu"].next()
                        wd_, wdt = R["wd"].next()
                        P.D("pool", [(wgu[:, :, 0, 0:ncs * 128], wgv[:, :, c0 * 128:(c0 + ncs) * 128]),
                                     (wgu[:, :, 1, 0:ncs * 128], wuv[:, :, c0 * 128:(c0 + ncs) * 128])], key=wgut, writes=[wgut])
                        P.D("pool", [(wd_[:, 0:ncs, :], wdv[:, c0:c0 + ncs, :])], key=wdt, writes=[wdt])
                        for blk in tset:
                            t0, n, s_ = blk
                            nt = n // 128
                            off = t0 - base_tok
                            aT, aTt = R["actT"].next()
                            for ch in range(ncs):
                                pg, pgt = ps_next()
                                pu, put = ps_next()
                                mm(pg[:, 0:n], [(wgu[:, k, 0, ch * 128:(ch + 1) * 128], hT2[:, k, off:off + n]) for k in range(KD)],
                                   reads=[wgut, ("hT2", t0)], writes=[pgt])
                                mm(pu[:, 0:n], [(wgu[:, k, 1, ch * 128:(ch + 1) * 128], hT2[:, k, off:off + n]) for k in range(KD)],
                                   reads=[wgut, ("hT2", t0)], writes=[put])
                                sg, sgt = R["tmpC"].next()
                                P.I("act", "activation", sg[:, 0:n], pg[:, 0:n], AF.Silu, reads=[pgt], writes=[sgt])
                                P.I("dve", "tensor_tensor", aT[:, ch, 0:n], sg[:, 0:n], pu[:, 0:n], ALU.mult, reads=[sgt, put], writes=[(aTt, ch)])
                            for ti in range(nt):
                                lt = off // 128 + ti
                                for cb in range(NF):
                                    py, pyt = ps_next()
                                    mm(py[:], [(aT[:, ch, ti * 128:(ti + 1) * 128], wd_[:, ch, cb * 512:(cb + 1) * 512]) for ch in range(ncs)],
                                       reads=[(aTt, ch) for ch in range(ncs)] + [wdt], writes=[pyt])
                                    ya = yacc[:, lt, cb * 512:(cb + 1) * 512]
                                    yat = ("yacc", lt, cb)
                                    if moe:
                                        gsc = gates[:, lt, e_:e_ + 1]
                                        if first:
                                            P.I("dve", "tensor_scalar", ya, py[:], gsc, None, ALU.mult, reads=[pyt, ("gates", lt)], writes=[yat])
                                        else:
                                            P.I("dve", "scalar_tensor_tensor", ya, py[:], gsc, ya, ALU.mult, ALU.add,
                                                reads=[pyt, ("gates", lt), yat], writes=[yat])
                                    else:
                                        if first:
                                            P.I("act", "activation", ya, py[:], AF.Copy, reads=[pyt], writes=[yat])
                                        else:
                                            P.I("dve", "tensor_tensor", ya, ya, py[:], ALU.add, reads=[pyt, yat], writes=[yat])
                        first = False
                P.barrier()
                ar["off"] = fixed_bytes
                for k_ in list(R.keys()):
                    del R[k_]
                for nm in (("xt", 4), ("r", 4), ("xo", 4)) + LN_RINGS:
                    make_ring(nm)
                for blk in tset:
                    t0, n, s_ = blk
                    if last and s_ == 1:
                        continue
                    grp = []
                    for ti in range(n // 128):
                        tile = t0 // 128 + ti
                        lt = (t0 - base_tok) // 128 + ti
                        if last:
                            dst, dtok = out_d[tile * 128:(tile + 1) * 128, :], ("out", tile)
                        else:
                            dst, dtok = x2_d[l][tile * 128:(tile + 1) * 128, :], ("x2_d", l, tile)
                        grp.append(((lambda cb, lt=lt: (yacc[:, lt, cb * 512:(cb + 1) * 512], ("yacc", lt, cb))),
                                    dst, dtok, x1_d[tile * 128:(tile + 1) * 128, :], ("x1_d", tile)))
                    post_norm_multi(grp, 2 + s_, 2, 3)

        KV_T = [("kT", [128, 2, TOT], BF16), ("V3", [128, TT, 2, 192], BF16)]
        for l in range(cfg.EMIT_LAYERS):
            last = (l == L - 1)
            ctx_out = not last
            phase(("wm",), [("scB", [128, 2, KD, 128], BF16), ("bmbc", [128, 2, D], F32)])
            modulation(l)
            if cfg.STOP == "mod":
                break
            wi = w_in[l].rearrange("(k p) n -> p k n", p=128)
            phase(HT_RINGS + ("tmpA", "tmpB", "tmpC", "tbf", "tbf2", "rcs"), KV_T + [("wA", [128, KD, 1024], BF16)])
            wA = T["wA"]
            P.I("pool", "memset", T["V3"][:, :, :, 64:128], 1.0, writes=["V3ones"])
            for h in range(2):
                for dup in range(2):
                    c0 = h * 128 + dup * 64
                    wload(wA[:, :, c0:c0 + 64], "wA", wi[:, :, cfg.OFF_K + h * 64:cfg.OFF_K + h * 64 + 64], key=("wA", h, dup))
            wload(wA[:, :, 256:384], "wA", wi[:, :, cfg.OFF_V:cfg.OFF_V + 128], key=("wA", 9))
            wload(wA[:, :, 512:1024], "wA", wi[:, :, cfg.OFF_FT:cfg.OFF_FT + 512], key=("wA", 10))
            for blk in lat_blocks + [ctx_block]:
                phase_A_block(l, blk, wi, ctx_out)
            q_blocks = lat_blocks + ([ctx_block] if ctx_out else [])
            if cfg.STOP == "A":
                break
            phase(("hTb", "yblk", "qT", "pT", "tmpA", "tmpB", "tmpC", "tbf", "tbf2", "rcs"),
                  KV_T + [("wA", [128, KD, 512], BF16), ("rsum", [128, 512], F32), ("bcs", [128, 512], F32), ("rhl", [128, 2, 512], BF16), ("sAB", [128, 2, 512], F32)])
            wload(T["wA"][:], "wA", wi[:, :, cfg.OFF_Q:cfg.OFF_Q + 512], key=("wA", 11))
            qs_cur = attn_prologue(q_blocks[0])
            for bi, blk in enumerate(q_blocks):
                nb_ = q_blocks[bi + 1] if bi + 1 < len(q_blocks) else None
                qs_cur = attn_block(blk, qs_cur, nb_)
            if cfg.STOP == "B1":
                break
            phase(("hTb", "yblk", "mx", "gv", "vg", "tmpA", "tmpB") + LN_RINGS,
                  [("wA", [128, KD, 1024], BF16), ("wB", [128, 4, 128], BF16), ("sgbias", [128, 512], F32)])
            wload(T["wA"][:, :, 0:512], "wA", wi[:, :, cfg.OFF_SGV:cfg.OFF_SGV + 512], key=("wA", 12))
            wload(T["wA"][:, :, 512:1024], "wA", wi[:, :, cfg.OFF_U:cfg.OFF_U + 512], key=("wA", 13))
            wload(T["wB"][:], "wB", sg_wT[l], key=("wB", 0))
            P.D("sp", [(T["sgbias"][:], sg_b[l:l + 1, :].broadcast_to([128, 512]))], key="sgbias", writes=["sgbias"])
            for blk in q_blocks:
                sg_block(blk)
            if cfg.STOP == "B2":
                break
            phase(("dft", "yfr"), [("ftok_sb", [128, NT, 512], BF16), ("Gsb", [128, 4, 2, 256], BF16)])
            fourier(0, NT, dftL_d, cfg.NKB, 256, 0)
            if ctx_out:
                fourier(NT, NCT, dftC_d, 1, CTX, S)
            if cfg.STOP == "B3":
                break
            phase(("hTb", "ybs0", "ybs1", "ybs2", "mT", "acc", "tmpA", "xt", "r", "xo") + LN_RINGS,
                  [("wA", [128, KD, 3 * D], BF16), ("wB", [128, 3, 4, D], BF16), ("wC", [128, KD, D], BF16)])
            wload(T["wA"][:], "wA", wi[:, :, cfg.OFF_GATE:cfg.OFF_GATE + 3 * D], key=("wA", 14))
            wload(T["wB"][:], "wB", w_branch[l].rearrange("n (c p) d -> p n c d", p=128), key=("wB", 1))
            wload(T["wC"][:], "wC", w_out[l].rearrange("(k p) d -> p k d", p=128), key=("wC", 0))
            ld_cur = merge_loads(q_blocks[0])
            for bi, blk in enumerate(q_blocks):
                nb_ = q_blocks[bi + 1] if bi + 1 < len(q_blocks) else None
                ld_cur = merge_block(l, blk, ld_cur, nb_)
            if cfg.STOP == "B4":
                break
            ffn(l, ctx_out, last)

        if cfg.MAXOPS is not None:
            print("total ops", len(P.ops))
            P.ops = P.ops[:cfg.MAXOPS]
            print("last op:", P.ops[-1].eng, P.ops[-1].reads, P.ops[-1].writes)
        stats = P.emit(final_waits=[("xo", i) for i in range(4)] if (cfg.STOP is None and cfg.MAXOPS is None) else [])
    return nc, stats


def make_in_maps(cfg, inputs, nb):
    D, KD = cfg.D, cfg.KD
    L = cfg.DEPTH
    consts = make_consts(cfg)
    f = lambda a: np.ascontiguousarray(np.asarray(a, dtype=np.float32))
    shared = dict(consts)
    shared["w_mod"] = f(inputs["w_mod"])
    bm = f(inputs["b_mod"])
    shared["b_mod"] = bm
    shared["b_modF"] = np.ascontiguousarray(bm.reshape(L, 6 * KD, 128).transpose(0, 2, 1))
    shared["w_in"] = f(inputs["w_in"])
    qn = np.tile(f(inputs["q_norm"]), (1, 2))
    kn = np.tile(f(inputs["k_norm"]), (1, 2))
    shared["qk_norm"] = np.ascontiguousarray(np.stack([qn, kn], axis=-1))
    shared["sg_wT"] = np.ascontiguousarray(f(inputs["sg_w"]).transpose(0, 3, 1, 2))
    shared["sg_b"] = np.ascontiguousarray(f(inputs["sg_b"]).reshape(L, 512))
    shared["w_branch"] = f(inputs["w_branch"])
    shared["w_out"] = f(inputs["w_out"])
    shared["ln_gb"] = np.ascontiguousarray(np.stack([f(inputs["ln1_g"]), f(inputs["ln1_b"]), f(inputs["ln2_g"]), f(inputs["ln2_b"])], axis=1))
    shared["ffn_w_gate"] = f(inputs["ffn_w_gate"])
    shared["ffn_w_up"] = f(inputs["ffn_w_up"])
    shared["ffn_w_down"] = f(inputs["ffn_w_down"])
    if L // 2:
        shared["router"] = f(inputs["router"])
        shared["exp_w_gate"] = f(inputs["exp_w_gate"])
        shared["exp_w_up"] = f(inputs["exp_w_up"])
        shared["exp_w_down"] = f(inputs["exp_w_down"])
    x = f(inputs["x"])
    ctx = f(inputs["ctx"])
    c = f(inputs["c"])
    cc = f(inputs["c_ctx"])
    maps = []
    for b in range(nb):
        m = dict(shared)
        m["x"] = x[b]
        m["ctx"] = ctx[b]
        cv = np.stack([c[b].reshape(KD, 128).T, cc.reshape(KD, 128).T], axis=-1)
        m["cvecs"] = np.ascontiguousarray(cv)
        maps.append(m)
    return maps


_CACHE = {}


def kernel(**inputs):
    cfg = FULL
    nb = inputs["x"].shape[0]
    if "nc" not in _CACHE:
        _CACHE["nc"] = build_program(cfg)[0]
    nc = _CACHE["nc"]
    maps = make_in_maps(cfg, inputs, nb)
    res = run_bass_kernel_spmd(nc, maps, core_ids=list(range(nb)))
    out = np.stack([np.asarray(r["out"], dtype=np.float32) for r in res.results], axis=0)
    return out
```
